# Optimizing a Trainium2 kernel written in Bass

```python
import jax, jax.numpy as jnp
from jax import lax
import numpy as np

D_MODEL = 2048
BATCH = 4
SEQ = 4096
DEPTH = 1

CHUNK = 64
SWA_Q_HEADS = 16
SWA_KV_HEADS = 4
SWA_HEAD_DIM = 64
SWA_GROUP = SWA_Q_HEADS // SWA_KV_HEADS
WINDOW = 128
WINDOW_CHUNKS = WINDOW // CHUNK
BAND = (WINDOW_CHUNKS + 1) * CHUNK
SWA_Q_W = SWA_Q_HEADS * SWA_HEAD_DIM
SWA_KV_W = SWA_KV_HEADS * SWA_HEAD_DIM
GDN_HEADS = 8
GDN_HEAD_DIM = 128
GDN_W = GDN_HEADS * GDN_HEAD_DIM
CONV_WIDTH = 4
IN_SIZES = (SWA_Q_W, SWA_KV_W, SWA_KV_W, GDN_W, GDN_W, GDN_W, GDN_W,
            GDN_HEADS, GDN_HEADS, D_MODEL, D_MODEL)
IN_WIDTH = sum(IN_SIZES)
N_GROUPS = 4
EXPERTS_PER_GROUP = 8
N_EXPERTS = N_GROUPS * EXPERTS_PER_GROUP
TOP_K = 2
EXPERT_FF = 512
DEEPNORM_ALPHA = (2 * DEPTH) ** 0.25
DEEPNORM_BETA = (8 * DEPTH) ** -0.25
LN_EPS = 1e-5
RMS_EPS = 1e-6
NEG_INF = -1e30

kernel_name = 'hybrid_swa_gdn_hier_moe_deepnorm_adaln'


def layer_norm(x, g=None, b=None):
    xf = x.astype(jnp.float32)
    mu = jnp.mean(xf, axis=-1, keepdims=True)
    var = jnp.mean(jnp.square(xf - mu), axis=-1, keepdims=True)
    y = (xf - mu) * lax.rsqrt(var + LN_EPS)
    if g is not None:
        y = y * g.astype(jnp.float32) + b.astype(jnp.float32)
    return y.astype(x.dtype)


def l2_normalize(x):
    return x * lax.rsqrt(jnp.sum(jnp.square(x), axis=-1, keepdims=True) + RMS_EPS)


def causal_depthwise_conv(x, w):
    s = x.shape[1]
    xp = jnp.pad(x, ((0, 0), (CONV_WIDTH - 1, 0), (0, 0)))
    y = xp[:, 0:s] * w[0]
    for i in range(1, CONV_WIDTH):
        y = y + xp[:, i:i + s] * w[i]
    return y


def swa_sink_attention(q, k, v, sinks):
    bsz, s, _ = q.shape
    n_c = s // CHUNK
    qf = q.astype(jnp.float32).reshape(bsz, n_c, CHUNK, SWA_KV_HEADS, SWA_GROUP, SWA_HEAD_DIM)
    pad = WINDOW_CHUNKS * CHUNK

    def band(t):
        t = t.astype(jnp.float32).reshape(bsz, s, SWA_KV_HEADS, SWA_HEAD_DIM)
        tp = jnp.pad(t, ((0, 0), (pad, 0), (0, 0), (0, 0)))
        tp = tp.reshape(bsz, n_c + WINDOW_CHUNKS, CHUNK, SWA_KV_HEADS, SWA_HEAD_DIM)
        return jnp.concatenate([tp[:, j:j + n_c] for j in range(WINDOW_CHUNKS + 1)], axis=2)

    kb = band(k)
    vb = band(v)
    key_chunk = jnp.arange(n_c)[:, None] - WINDOW_CHUNKS + (jnp.arange(BAND) // CHUNK)[None, :]
    valid = key_chunk >= 0
    scores = jnp.einsum('bnqhgd,bnkhd->bnhgqk', qf, kb) * (SWA_HEAD_DIM ** -0.5)
    scores = jnp.where(valid[None, :, None, None, None, :], scores, NEG_INF)
    sink = sinks.astype(jnp.float32).reshape(1, 1, SWA_KV_HEADS, SWA_GROUP, 1, 1)
    sink = jnp.broadcast_to(sink, scores.shape[:-1] + (1,))
    probs = jax.nn.softmax(jnp.concatenate([scores, sink], axis=-1), axis=-1)[..., :-1]
    out = jnp.einsum('bnhgqk,bnkhd->bnqhgd', probs, vb)
    return out.reshape(bsz, s, SWA_Q_W).astype(v.dtype)


def gated_delta_rule(q, k, v, g, beta):
    bsz, s, h, dk = q.shape
    dv = v.shape[-1]
    n_c = s // CHUNK

    def to_chunks(t):
        return t.reshape(bsz, n_c, CHUNK, h, -1).transpose(0, 3, 1, 2, 4)

    q, k, v = to_chunks(q), to_chunks(k), to_chunks(v)
    g = g.reshape(bsz, n_c, CHUNK, h).transpose(0, 3, 1, 2)
    beta = beta.reshape(bsz, n_c, CHUNK, h).transpose(0, 3, 1, 2)
    cum = jnp.cumsum(g, axis=-1)
    idx = jnp.arange(CHUNK)
    strict = idx[:, None] > idx[None, :]
    incl = idx[:, None] >= idx[None, :]
    diff = cum[..., :, None] - cum[..., None, :]
    decay_incl = jnp.where(incl, jnp.exp(jnp.where(incl, diff, 0.0)), 0.0)
    kk = jnp.einsum('bhncd,bhnjd->bhncj', k, k)
    a_mat = jnp.where(strict, beta[..., :, None] * kk * decay_incl, 0.0)
    eye = jnp.eye(CHUNK, dtype=jnp.float32)
    t_mat = lax.linalg.triangular_solve(a_mat + eye, jnp.broadcast_to(eye, a_mat.shape),
                                        left_side=True, lower=True, unit_diagonal=True)
    u = t_mat @ (v * beta[..., None])
    w = t_mat @ (k * (beta * jnp.exp(cum))[..., None])
    qk = jnp.einsum('bhncd,bhnjd->bhncj', q, k) * decay_incl
    q_dec = q * jnp.exp(cum)[..., None]
    k_dec = k * jnp.exp(cum[..., -1:] - cum)[..., None]
    chunk_decay = jnp.exp(cum[..., -1])

    def step(state, xs):
        u_c, w_c, qk_c, qd_c, kd_c, dec_c = xs
        v_new = u_c - w_c @ state
        o_c = qd_c @ state + qk_c @ v_new
        state = dec_c[..., None, None] * state + jnp.swapaxes(kd_c, -1, -2) @ v_new
        return state, o_c

    xs = (jnp.moveaxis(u, 2, 0), jnp.moveaxis(w, 2, 0), jnp.moveaxis(qk, 2, 0),
          jnp.moveaxis(q_dec, 2, 0), jnp.moveaxis(k_dec, 2, 0), jnp.moveaxis(chunk_decay, 2, 0))
    state0 = jnp.zeros((bsz, h, dk, dv), jnp.float32)
    _, o = lax.scan(step, state0, xs)
    return o.transpose(1, 0, 3, 2, 4).reshape(bsz, s, h, dv)


def token_mixer(u, w_in, conv_w, swa_sinks, gdn_a_log, gdn_dt_bias, gdn_norm_w,
                w_proj_a, w_proj_b, w_out):
    bsz, s, _ = u.shape
    hcat = u @ w_in
    split_points = tuple(np.cumsum(IN_SIZES)[:-1].tolist())
    qa, ka, va, qb, kb, vb, zb, b_logit, a_logit, ga_logit, gb_logit = jnp.split(hcat, split_points, axis=-1)
    oa = swa_sink_attention(qa, ka, va, swa_sinks)
    qkv = jax.nn.silu(causal_depthwise_conv(jnp.concatenate([qb, kb, vb], axis=-1), conv_w))
    qb, kb, vb = jnp.split(qkv, 3, axis=-1)
    shp = (bsz, s, GDN_HEADS, GDN_HEAD_DIM)
    qh = l2_normalize(qb.astype(jnp.float32).reshape(shp)) * (GDN_HEAD_DIM ** -0.5)
    kh = l2_normalize(kb.astype(jnp.float32).reshape(shp))
    vh = vb.astype(jnp.float32).reshape(shp)
    beta = jax.nn.sigmoid(b_logit.astype(jnp.float32))
    g = -jnp.exp(gdn_a_log.astype(jnp.float32)) * jax.nn.softplus(
        a_logit.astype(jnp.float32) + gdn_dt_bias.astype(jnp.float32))
    ob = gated_delta_rule(qh, kh, vh, g, beta)
    ob = ob * lax.rsqrt(jnp.mean(jnp.square(ob), axis=-1, keepdims=True) + RMS_EPS)
    ob = ob * gdn_norm_w.astype(jnp.float32) * jax.nn.silu(zb.astype(jnp.float32).reshape(shp))
    ob = ob.reshape(bsz, s, GDN_W).astype(u.dtype)
    merged = jax.nn.sigmoid(ga_logit) * (oa @ w_proj_a) + jax.nn.sigmoid(gb_logit) * (ob @ w_proj_b)
    return merged @ w_out


def hier_moe(u, w_router_group, b_router_group, w_router_expert, b_router_expert, w_gate_up, w_down):
    bsz, s, d = u.shape
    t = u.reshape(-1, d)
    n = t.shape[0]
    p_group = jax.nn.softmax((t @ w_router_group + b_router_group).astype(jnp.float32), axis=-1)
    g_idx = jnp.argmax(p_group, axis=-1)
    p_group_sel = jnp.take_along_axis(p_group, g_idx[:, None], axis=-1)
    e_logits = (t @ w_router_expert + b_router_expert).astype(jnp.float32)
    e_logits = e_logits.reshape(n, N_GROUPS, EXPERTS_PER_GROUP)
    e_logits = jnp.take_along_axis(e_logits, g_idx[:, None, None], axis=1)[:, 0]
    p_exp = jax.nn.softmax(e_logits, axis=-1)
    top_p, top_i = lax.top_k(p_exp, TOP_K)
    top_p = top_p / jnp.sum(top_p, axis=-1, keepdims=True)
    weights = p_group_sel * top_p
    expert_idx = g_idx[:, None] * EXPERTS_PER_GROUP + top_i
    combine = jnp.sum(jax.nn.one_hot(expert_idx, N_EXPERTS, dtype=jnp.float32) * weights[..., None], axis=1)
    y = jnp.zeros((n, d), jnp.float32)
    for e in range(N_EXPERTS):
        gate, up = jnp.split(t @ w_gate_up[e], 2, axis=-1)
        y = y + combine[:, e:e + 1] * ((jax.nn.silu(gate) * up) @ w_down[e]).astype(jnp.float32)
    return y.reshape(bsz, s, d).astype(u.dtype)


def setup_inputs(seed: int = 0) -> dict:
    key = jax.random.key(seed)
    ks = jax.random.split(key, 24)
    f32 = jnp.float32
    nrm = lambda k, shape, scale: jax.random.normal(k, shape, f32) * scale
    L = DEPTH
    dt = jnp.exp(jax.random.uniform(ks[7], (L, GDN_HEADS), f32, np.log(1e-3), np.log(1e-1)))
    return {
        'x': nrm(ks[0], (BATCH, SEQ, D_MODEL), 1.0),
        'c': nrm(ks[1], (BATCH, D_MODEL), 1.0),
        'w_ada': nrm(ks[2], (L, D_MODEL, 6 * D_MODEL), 0.5 * D_MODEL ** -0.5),
        'b_ada': nrm(ks[3], (L, 6 * D_MODEL), 0.02),
        'w_in': nrm(ks[4], (L, D_MODEL, IN_WIDTH), D_MODEL ** -0.5),
        'conv_w': nrm(ks[5], (L, CONV_WIDTH, 3 * GDN_W), CONV_WIDTH ** -0.5),
        'swa_sinks': nrm(ks[6], (L, SWA_Q_HEADS), 0.5),
        'gdn_a_log': jnp.log(jax.random.uniform(ks[8], (L, GDN_HEADS), f32, 1.0, 16.0)),
        'gdn_dt_bias': dt + jnp.log(-jnp.expm1(-dt)),
        'gdn_norm_w': 1.0 + nrm(ks[9], (L, GDN_HEAD_DIM), 0.1),
        'w_proj_a': nrm(ks[10], (L, SWA_Q_W, D_MODEL), DEEPNORM_BETA * SWA_Q_W ** -0.5),
        'w_proj_b': nrm(ks[11], (L, GDN_W, D_MODEL), DEEPNORM_BETA * GDN_W ** -0.5),
        'w_out': nrm(ks[12], (L, D_MODEL, D_MODEL), DEEPNORM_BETA * D_MODEL ** -0.5),
        'ln1_g': 1.0 + nrm(ks[13], (L, D_MODEL), 0.1),
        'ln1_b': nrm(ks[14], (L, D_MODEL), 0.02),
        'w_router_group': nrm(ks[15], (L, D_MODEL, N_GROUPS), D_MODEL ** -0.5),
        'b_router_group': nrm(ks[16], (L, N_GROUPS), 0.01),
        'w_router_expert': nrm(ks[17], (L, D_MODEL, N_EXPERTS), D_MODEL ** -0.5),
        'b_router_expert': nrm(ks[18], (L, N_EXPERTS), 0.01),
        'w_gate_up': nrm(ks[19], (L, N_EXPERTS, D_MODEL, 2 * EXPERT_FF), D_MODEL ** -0.5),
        'w_down': nrm(ks[20], (L, N_EXPERTS, EXPERT_FF, D_MODEL), DEEPNORM_BETA * EXPERT_FF ** -0.5),
        'ln2_g': 1.0 + nrm(ks[21], (L, D_MODEL), 0.1),
        'ln2_b': nrm(ks[22], (L, D_MODEL), 0.02),
    }


def reference(x, c, w_ada, b_ada, w_in, conv_w, swa_sinks, gdn_a_log, gdn_dt_bias, gdn_norm_w,
              w_proj_a, w_proj_b, w_out, ln1_g, ln1_b, w_router_group, b_router_group,
              w_router_expert, b_router_expert, w_gate_up, w_down, ln2_g, ln2_b):
    for l in range(DEPTH):
        mod = (jax.nn.silu(c) @ w_ada[l] + b_ada[l])[:, None, :]
        sh1, sc1, g1, sh2, sc2, g2 = jnp.split(mod, 6, axis=-1)
        u = layer_norm(x) * (1.0 + sc1) + sh1
        mix = token_mixer(u, w_in[l], conv_w[l], swa_sinks[l], gdn_a_log[l], gdn_dt_bias[l],
                          gdn_norm_w[l], w_proj_a[l], w_proj_b[l], w_out[l])
        x = layer_norm(DEEPNORM_ALPHA * x + g1 * mix, ln1_g[l], ln1_b[l])
        u = layer_norm(x) * (1.0 + sc2) + sh2
        ffn = hier_moe(u, w_router_group[l], b_router_group[l], w_router_expert[l],
                       b_router_expert[l], w_gate_up[l], w_down[l])
        x = layer_norm(DEEPNORM_ALPHA * x + g2 * ffn, ln2_g[l], ln2_b[l])
    return x
```

```python
import contextlib
import numpy as np
import concourse.bass as bass
import concourse.mybir as mybir
from concourse.bass_utils import run_bass_kernel_spmd

F32 = mybir.dt.float32
BF16 = mybir.dt.bfloat16
AF = mybir.ActivationFunctionType
ALU = mybir.AluOpType
AX = mybir.AxisListType

D = 2048
KC = 16
NEXP = 32
ALPHA = 2.0 ** 0.25
LN_EPS = 1e-5
RMS_EPS = 1e-6
NEG = -30000.0
PW = 256
DEBUG = {}


class Sched:
    def __init__(self, nc, es):
        self.nc = nc
        self.es = es
        self.eng = {"pe": nc.tensor, "act": nc.scalar, "dve": nc.vector, "pool": nc.gpsimd, "sp": nc.sync}
        self.cnt = {}
        self.sem = {}
        self.nsem = 0
        self.lastw = {}
        self.readers = {}
        self.seen = {e: {} for e in self.eng}
        self.slots = {}
        self.ninst = 0
        for e in ("pe", "act", "dve", "pool"):
            self._newsem(e)

    def _newsem(self, e):
        self.nsem += 1
        s = self.es.enter_context(self.nc.semaphore(f"s_{e}_{self.nsem}"))
        self.sem[e] = (s, self.nsem)
        self.cnt[e] = 0

    def _wait(self, e, deps):
        best = {}
        for (s, sid, val, se) in deps:
            if se == e and e == "pe":
                continue
            if best.get(sid, (None, 0))[1] < val:
                best[sid] = (s, val)
        seen = self.seen[e]
        for sid, (s, val) in best.items():
            if seen.get(sid, 0) >= val:
                continue
            self.eng[e].wait_ge(s, val)
            seen[sid] = val

    def _collect(self, r, w, e=None):
        deps = []
        for k in r:
            x = self.lastw.get(k)
            if x is not None:
                deps.append(x)
            if k.startswith("ps"):
                rd = self.readers.get(k)
                if rd:
                    deps.extend(v for v in rd.values() if v[3] != e)
        for k in w:
            x = self.lastw.get(k)
            if x is not None:
                deps.append(x)
            rd = self.readers.get(k)
            if rd:
                deps.extend(rd.values())
        return deps

    def _record(self, ev, r, w):
        for k in r:
            d = self.readers.setdefault(k, {})
            d[ev[1]] = ev
        for k in w:
            self.lastw[k] = ev
            self.readers[k] = {}

    def op(self, e, fn, r=(), w=()):
        self._wait(e, self._collect(r, w, e))
        ins = fn(self.eng[e])
        if self.cnt[e] >= 40000:
            self._newsem(e)
        s, sid = self.sem[e]
        self.cnt[e] += 1
        ins.then_inc(s, 1)
        ev = (s, sid, self.cnt[e], e)
        self._record(ev, r, w)
        self.ninst += 1
        return ev

    def dma(self, e, out, in_, slot, r=(), w=()):
        self._wait(e, self._collect(r, w, e))
        if slot not in self.slots:
            self.nsem += 1
            s = self.es.enter_context(self.nc.semaphore(f"d_{self.nsem}"))
            self.slots[slot] = [s, self.nsem, 0]
        sl = self.slots[slot]
        self.eng[e].dma_start(out=out, in_=in_).then_inc(sl[0], 16)
        sl[2] += 16
        ev = (sl[0], sl[1], sl[2], "dma")
        self._record(ev, r, w)
        self.ninst += 1
        return ev

    def barrier(self):
        evs = []
        for e in ("pe", "act", "dve", "pool"):
            s, sid = self.sem[e]
            if self.cnt[e] > 0:
                evs.append((s, sid, self.cnt[e], e))
        for slot, (s, sid, val) in self.slots.items():
            if val > 0:
                evs.append((s, sid, val, "dma"))
        for e in self.eng:
            self._wait(e, evs)
        self.lastw.clear()
        self.readers.clear()


def build_nc(NSBP, NSBM, NG, dbg=False):
    TOKM = NSBM * 512
    TOKP = NSBP * 512
    NTB = TOKM // 128
    nc = bass.Bass("TRN2", target_bir_lowering=False)

    def din(name, shape, dt=F32):
        return nc.dram_tensor(name, list(shape), dt, kind="ExternalInput").ap()

    xm = din("xm", [TOKM, D])
    xp = din("xp", [TOKP, D])
    cT = din("cT", [128, KC])
    wada = din("wada", [48, 128, KC, PW])
    badaT = din("badaT", [128, 96])
    badag = din("badag", [2, D])
    win = din("win", [39, 128, KC, PW])
    wbd = din("wbd", [128, KC, 16])
    convw = din("convw", [128, 24, 4])
    sinks = din("sinks", [16])
    alog = din("alog", [8])
    dtb = din("dtb", [8])
    normw = din("normw", [128])
    wproj = din("wproj", [8, 128, KC, PW])
    wout = din("wout", [8, 128, KC, PW])
    lnv = din("lnv", [4, D])
    wr = din("wr", [128, KC, 36])
    br = din("br", [36])
    wexp = din("wexp", [NEXP, 6, 128, KC, PW])
    consts = din("consts", [128, 5 * 128 + 4])
    x1s = nc.dram_tensor("x1s", [TOKM, D], F32, kind="Internal").ap()
    gscr = nc.dram_tensor("gscr", [2, 128, D], F32, kind="Internal").ap()
    out = nc.dram_tensor("out", [TOKM, D], F32, kind="ExternalOutput").ap()
    dbg_out = {}

    with contextlib.ExitStack() as es:
        S = Sched(nc, es)

        def sb(name, shape, dt=F32):
            return es.enter_context(nc.sbuf_tensor(name, list(shape), dt))

        PS = [es.enter_context(nc.psum_tensor(f"ps{i}", [128, 512], F32)) for i in range(8)]
        psi = [0]

        def psn():
            i = psi[0]
            psi[0] = (i + 1) % 8
            return PS[i], f"ps{i}"

        NT32 = 6
        T32 = [sb(f"t32_{i}", [128, 512], F32) for i in range(NT32)]
        tix = [0, 0]

        def t32():
            i = tix[0]
            tix[0] = (i + 1) % NT32
            return T32[i], f"t32_{i}"

        cst = sb("cst", [128, 5 * 128 + 4])
        S.dma("sp", cst[:], consts, "cst", w=["cst"])
        ident = cst[:, 0:128]
        UT = cst[:, 128:256]
        ONB = cst[:, 256:384]
        NEGM = cst[:, 384:512]
        SM = cst[:, 512:640]
        pv = cst[:, 640:641]
        kbias = cst[:, 641:642]
        cm0 = cst[:, 642:643]
        cm1 = cst[:, 643:644]
        identb = sb("identb", [128, 128], BF16)
        S.op("dve", lambda e: e.tensor_copy(out=identb[:], in_=ident), r=["cst"], w=["identb"])
        ones32 = sb("ones32", [128, 128])
        S.op("dve", lambda e: e.memset(ones32[:], 1.0), w=["ones32"])
        epsln = sb("epsln", [128, 1])
        S.op("dve", lambda e: e.memset(epsln[:], LN_EPS), w=["epsln"])
        epsrms = sb("epsrms", [128, 1])
        S.op("dve", lambda e: e.memset(epsrms[:], RMS_EPS), w=["epsrms"])

        small = sb("small", [128, 16 + 8 + 8 + 128 + 36])
        esink = small[:, 0:16]
        negA = small[:, 16:24]
        dtbb = small[:, 24:32]
        normwb = small[:, 32:160]
        brb = small[:, 160:196]
        S.dma("sp", esink, sinks.partition_broadcast(128), "sm", w=["small"])
        S.dma("sp", negA, alog.partition_broadcast(128), "sm", w=["small"])
        S.dma("sp", dtbb, dtb.partition_broadcast(128), "sm", w=["small"])
        S.dma("sp", normwb, normw.partition_broadcast(128), "sm", w=["small"])
        S.dma("sp", brb, br.partition_broadcast(128), "sm", w=["small"])
        S.op("act", lambda e: e.activation(out=esink, in_=esink, func=AF.Exp), r=["small"], w=["small"])
        S.op("act", lambda e: e.activation(out=negA, in_=negA, func=AF.Exp), r=["small"], w=["small"])
        S.op("dve", lambda e: e.tensor_scalar(out=negA, in0=negA, scalar1=-1.0, scalar2=None, op0=ALU.mult),
             r=["small"], w=["small"])
        convw_sb = sb("convw_sb", [128, 24, 4])
        S.dma("sp", convw_sb[:], convw, "cw", w=["convw"])
        wbd_sb = sb("wbd_sb", [128, KC, 16], BF16)
        S.dma("pool", wbd_sb[:], wbd, "wbd", w=["wbd"])
        wr_sb = sb("wr_sb", [128, KC, 36])
        S.dma("sp", wr_sb[:], wr, "wr", w=["wr"])

        NW = 3
        WP = [sb(f"wp{i}", [128, KC, PW], BF16) for i in range(NW)]
        wpi = [0]

        def load_panel(src):
            i = wpi[0]
            wpi[0] = (i + 1) % NW
            S.dma("pool", WP[i][:], src, f"wp{i}", w=[f"wp{i}"])
            return WP[i], f"wp{i}"

        XT = [sb(f"xt{i}", [128, D]) for i in range(2)]
        xti = [0]
        modT = sb("modT", [128, 96])
        es0 = contextlib.ExitStack()

        def sb0(name, shape, dt=F32):
            return es0.enter_context(nc.sbuf_tensor(name, list(shape), dt))

        bT = sb0("bT", [128, 96])
        S.dma("sp", bT[:], badaT, "bT", w=["bT"])
        cTs = sb0("cTs", [128, KC])
        S.dma("sp", cTs[:], cT, "cT", w=["cTs"])
        scb = sb0("scb", [128, KC], BF16)
        S.op("act", lambda e: e.activation(out=scb[:], in_=cTs[:], func=AF.Silu), r=["cTs"], w=["scb"])
        screp = sb0("screp", [128, KC, 128], BF16)
        S.op("dve", lambda e: e.tensor_copy(out=screp[:], in_=scb[:].unsqueeze(2).to_broadcast([128, KC, 128])),
             r=["scb"], w=["screp"])
        gbc = [XT[0], XT[1]]
        S.dma("sp", XT[0][:], badag[0].partition_broadcast(128), "xt0", w=["xt0"])
        S.dma("sp", XT[1][:], badag[1].partition_broadcast(128), "xt1", w=["xt1"])
        for pi in range(48):
            wp, wk = load_panel(wada[pi])
            ps, pk = psn()
            for jj in range(2):
                for k in range(KC):
                    S.op("pe", lambda e, jj=jj, k=k: e.matmul(ps[:, jj:jj + 1], wp[:, k, jj * 128:(jj + 1) * 128],
                                                             scb[:, k:k + 1], start=(k == 0), stop=(k == KC - 1)),
                         r=[wk, "scb"], w=[pk])
            S.op("dve", lambda e: e.tensor_tensor(out=modT[:, pi * 2:pi * 2 + 2], in0=ps[:, 0:2],
                                                  in1=bT[:, pi * 2:pi * 2 + 2], op=ALU.add),
                 r=[pk, "bT"], w=["modT"])
            gi = None
            if 16 <= pi < 24:
                gi, off = 0, (pi - 16) * PW
            elif 40 <= pi < 48:
                gi, off = 1, (pi - 40) * PW
            if gi is not None:
                ps2, pk2 = psn()
                for k in range(KC):
                    S.op("pe", lambda e, k=k: e.matmul(ps2[:, 0:PW], screp[:, k, :], wp[:, k, :],
                                                       start=(k == 0), stop=(k == KC - 1)),
                         r=[wk, "screp"], w=[pk2])
                S.op("dve", lambda e: e.tensor_tensor(out=gbc[gi][:, off:off + PW], in0=ps2[:, 0:PW],
                                                      in1=gbc[gi][:, off:off + PW], op=ALU.add),
                     r=[pk2, f"xt{gi}"], w=[f"xt{gi}"])
        S.op("dve", lambda e: e.tensor_scalar(out=modT[:, 16:32], in0=modT[:, 16:32], scalar1=1.0, scalar2=None,
                                              op0=ALU.add), r=["modT"], w=["modT"])
        S.op("dve", lambda e: e.tensor_scalar(out=modT[:, 64:80], in0=modT[:, 64:80], scalar1=1.0, scalar2=None,
                                              op0=ALU.add), r=["modT"], w=["modT"])
        sh1, sc1, sh2, sc2 = modT[:, 0:16], modT[:, 16:32], modT[:, 48:64], modT[:, 64:80]
        for gi in range(2):
            S.dma("sp", gscr[gi], XT[gi][:], "gscr", r=[f"xt{gi}"], w=["gscr"])
        S.barrier()
        es0.close()

        stt = sb("stt", [128, 4, 6])
        mv = sb("mv", [128, 4])

        def ln_stats(xa, xk):
            for c in range(4):
                S.op("dve", lambda e, c=c: e.bn_stats(out=stt[:, c, :], in_=xa[:, c * 512:(c + 1) * 512]),
                     r=[xk], w=["stt"])
            S.op("dve", lambda e: e.bn_aggr(out=mv[:, 0:2], in_=stt[:].rearrange("p a b -> p (a b)")),
                 r=["stt"], w=["mv"])
            S.op("act", lambda e: e.activation(out=mv[:, 2:3], in_=mv[:, 1:2], func=AF.Sqrt, bias=epsln[:], scale=1.0),
                 r=["mv", "epsln"], w=["mv"])
            S.op("dve", lambda e: e.reciprocal(out=mv[:, 2:3], in_=mv[:, 2:3]), r=["mv"], w=["mv"])

        def ln_normalize(xa, xk, oa, ok):
            S.op("dve", lambda e: e.tensor_scalar(out=oa, in0=xa, scalar1=mv[:, 0:1], scalar2=mv[:, 2:3],
                                                  op0=ALU.subtract, op1=ALU.mult), r=[xk, "mv"], w=[ok])

        def mod_transpose(xa, xk, sc, sh, dst, dk, dst32=None, d32k=None):
            for k4 in range(4):
                ps, pk = psn()
                for q in range(4):
                    k = k4 * 4 + q
                    S.op("pe", lambda e, k=k, q=q: e.transpose(ps[:, q * 128:(q + 1) * 128],
                                                               xa[:, k * 128:(k + 1) * 128], ident),
                         r=[xk, "cst"], w=[pk])
                for q in range(4):
                    k = k4 * 4 + q
                    if dst32 is not None:
                        S.op("dve", lambda e, k=k, q=q: e.tensor_scalar(
                            out=dst32(k), in0=ps[:, q * 128:(q + 1) * 128], scalar1=sc[:, k:k + 1],
                            scalar2=sh[:, k:k + 1], op0=ALU.mult, op1=ALU.add), r=[pk, "modT"], w=[d32k])
                        S.op("act", lambda e, k=k: e.copy(out=dst(k), in_=dst32(k)), r=[d32k], w=[dk])
                    elif q % 2 == 0:
                        S.op("act", lambda e, k=k, q=q: e.activation(
                            out=dst(k), in_=ps[:, q * 128:(q + 1) * 128], func=AF.Identity,
                            bias=sh[:, k:k + 1], scale=sc[:, k:k + 1]), r=[pk, "modT"], w=[dk])
                    else:
                        S.op("dve", lambda e, k=k, q=q: e.tensor_scalar(
                            out=dst(k), in0=ps[:, q * 128:(q + 1) * 128], scalar1=sc[:, k:k + 1],
                            scalar2=sh[:, k:k + 1], op0=ALU.mult, op1=ALU.add), r=[pk, "modT"], w=[dk])

        import os as _os
        _stop = _os.environ.get("K_STOP", "")
        es1 = contextlib.ExitStack()
        sb_outer = sb

        def sb(name, shape, dt=F32):
            return es1.enter_context(nc.sbuf_tensor(name, list(shape), dt))

        g1bc = sb("g1bc", [128, D])
        S.dma("sp", g1bc[:], gscr[0], "g1bc", r=["gscr"], w=["g1bc"])
        uT = sb("uT", [128, KC, 512], BF16)
        QT = sb("QT", [128, 8, 512], BF16)
        KT = sb("KT", [128, 4, 640], BF16)
        VA = sb("VA", [128, 5, 4, 65], BF16)
        S.op("dve", lambda e: e.memset(VA[:], 1.0), w=["VA"])
        S.op("dve", lambda e: e.memset(KT[:], 0.0), w=["KT"])
        hist = sb("hist", [128, 24, 3])
        S.op("dve", lambda e: e.memset(hist[:], 0.0), w=["hist"])
        qkm = sb("qkm", [128, KC, 512], BF16)
        qT = qkm[:, 0:8, :]
        kT = qkm[:, 8:16, :]
        mT = qkm
        vT = sb("vT", [128, 8, 512], BF16)
        zs = sb("zs", [128, 4, 1024], BF16)
        bd = sb("bd", [128, 4, 16])
        S32 = sb("S32", [128, 8, 128])
        Sb = [sb(f"Sb{i}", [128, 8, 128], BF16) for i in range(2)]
        S.op("dve", lambda e: e.memset(S32[:], 0.0), w=["S32"])
        S.op("dve", lambda e: e.memset(Sb[0][:], 0.0), w=["Sb0"])
        sbi = [0]
        PT = [sb(f"PT{i}", [128, 4, 128], BF16) for i in range(4)]
        for i in range(4):
            S.op("dve", lambda e, i=i: e.memset(PT[i][:], 0.0), w=[f"PT{i}"])
        oaun = sb("oaun", [128, 16, 65])
        oa = sb("oa", [128, 1024], BF16)
        ob = sb("ob", [128, 1024], BF16)
        oaT = QT
        obT = vT
        pre = [sb(f"pre{i}", [128, 515]) for i in range(2)]
        prei = [0]
        gsm = sb("gsm", [128, 96])
        vtok = sb("vtok", [128, 8, 128], BF16)
        kdec = sb("kdec", [128, 8, 128], BF16)
        kdd = sb("kdd", [128, 8, 128], BF16)
        nwT = sb("nwT", [128, 8, 128], BF16)
        up = sb("up", [128, 8, 128])
        vn = sb("vn", [128, 8, 128], BF16)
        oacc = sb("oacc", [128, 8, 128])
        Xb = [sb(f"Xb{i}", [128, 4, 128], BF16) for i in range(4)]
        Yb = [sb(f"Yb{i}", [128, 4, 128], BF16) for i in range(4)]
        Pb = [sb(f"Pb{i}", [128, 4, 128], BF16) for i in range(4)]
        QKm = [sb(f"QKm{i}", [128, 4, 128], BF16) for i in range(2)]

        def dump(name, ap_sb, key, shape):
            if not dbg:
                return
            t = nc.dram_tensor("dbg_" + name, list(shape), F32, kind="ExternalOutput").ap()
            tmp = sb("dbgt_" + name, list(shape))
            S.op("dve", lambda e: e.tensor_copy(out=tmp[:], in_=ap_sb), r=[key], w=["dbgt_" + name])
            S.dma("sp", t, tmp[:], "dbg_" + name, r=["dbgt_" + name])
            dbg_out[name] = True

        if dbg:
            dump("modT", modT[:], "modT", [128, 96])

        def mixer_sb(xsrc, is_prefix, need_kv, first_main, sbi_main):
            for tb in range(4):
                xa = XT[xti[0]]
                xk = f"xt{xti[0]}"
                xti[0] ^= 1
                S.dma("sp", xa[:], xsrc[tb * 128:(tb + 1) * 128, :], xk, w=[xk])
                ln_stats(xa[:], xk)
                ln_normalize(xa[:], xk, xa[:], xk)
                mod_transpose(xa[:], xk, sc1, sh1, lambda k, tb=tb: uT[:, k, tb * 128:(tb + 1) * 128], "uT")

            _st2 = _os.environ.get("K_STOP2", "")
            if _st2 == "s1":
                return

            def fm_tile(panel, col, evac):
                wp, wk = panel
                ps, pk = psn()
                for k in range(KC):
                    S.op("pe", lambda e, k=k: e.matmul(ps[:], wp[:, k, col:col + 128], uT[:, k, :],
                                                       start=(k == 0), stop=(k == KC - 1)), r=[wk, "uT"], w=[pk])
                evac(ps, pk)

            if not is_prefix:
                for p in range(4):
                    pan = load_panel(win[p])
                    for t in range(2):
                        c = p * 2 + t
                        fm_tile(pan, t * 128, lambda ps, pk, c=c: S.op(
                            "act", lambda e: e.copy(out=QT[:, c, :], in_=ps[:]), r=[pk], w=["QT"]))
            if need_kv:
                for p in range(2):
                    pan = load_panel(win[4 + p])
                    for t in range(2):
                        h = p * 2 + t
                        fm_tile(pan, t * 128, lambda ps, pk, h=h: S.op(
                            "dve", lambda e: e.tensor_copy(out=KT[:, h, 128:640], in_=ps[:]), r=[pk], w=["KT"]))
                wp, wk = load_panel(win[6])
                for tb in range(4):
                    ps, pk = psn()
                    for k in range(KC):
                        S.op("pe", lambda e, k=k, tb=tb: e.matmul(ps[:, 0:256], uT[:, k, tb * 128:(tb + 1) * 128],
                                                                  wp[:, k, :], start=(k == 0), stop=(k == KC - 1)),
                             r=[wk, "uT"], w=[pk])
                    S.op("act", lambda e, tb=tb: e.copy(out=VA[:, 1 + tb, :, 0:64],
                                                        in_=ps[:, 0:256].rearrange("p (h d) -> p h d", h=4)),
                         r=[pk], w=["VA"])
            if _st2 == "s2":
                return
            for tb in range(4):
                ps, pk = psn()
                for k in range(KC):
                    S.op("pe", lambda e, k=k, tb=tb: e.matmul(ps[:, 0:16], uT[:, k, tb * 128:(tb + 1) * 128],
                                                              wbd_sb[:, k, :], start=(k == 0), stop=(k == KC - 1)),
                         r=["wbd", "uT"], w=[pk])
                S.op("dve", lambda e, tb=tb: e.tensor_copy(out=bd[:, tb, :], in_=ps[:, 0:16]), r=[pk], w=["bd"])

            if _st2 == "s3":
                return
            for ft in range(24):
                if is_prefix and ft < 8:
                    continue
                if ft % 2 == 0:
                    pan = load_panel(win[7 + ft // 2])
                kind = ft // 8
                hh = ft % 8

                def evac(ps, pk, ft=ft, kind=kind, hh=hh):
                    pr = pre[prei[0]]
                    prk = f"pre{prei[0]}"
                    prei[0] ^= 1
                    S.op("dve", lambda e: e.tensor_copy(out=pr[:, 0:3], in_=hist[:, ft, :]), r=["hist"], w=[prk])
                    if is_prefix:
                        S.op("act", lambda e: e.activation(out=pr[:, 3:515], in_=ps[:], func=AF.Copy, scale=pv),
                             r=[pk, "cst"], w=[prk])
                    else:
                        S.op("act", lambda e: e.copy(out=pr[:, 3:515], in_=ps[:]), r=[pk], w=[prk])
                    S.op("dve", lambda e: e.tensor_copy(out=hist[:, ft, :], in_=pr[:, 512:515]), r=[prk], w=["hist"])
                    y, yk = t32()
                    S.op("dve", lambda e: e.tensor_scalar(out=y[:], in0=pr[:, 0:512], scalar1=convw_sb[:, ft, 0:1],
                                                          scalar2=None, op0=ALU.mult), r=[prk, "convw"], w=[yk])
                    for i in range(1, 4):
                        S.op("dve", lambda e, i=i: e.scalar_tensor_tensor(
                            out=y[:], in0=pr[:, i:i + 512], scalar=convw_sb[:, ft, i:i + 1], in1=y[:],
                            op0=ALU.mult, op1=ALU.add), r=[prk, "convw", yk], w=[yk])
                    if kind == 2:
                        S.op("act", lambda e: e.activation(out=vT[:, hh, :], in_=y[:], func=AF.Silu), r=[yk], w=["vT"])
                        return
                    sl, slk = t32()
                    S.op("act", lambda e: e.activation(out=sl[:], in_=y[:], func=AF.Silu), r=[yk], w=[slk])
                    sq, sqk = t32()
                    S.op("dve", lambda e: e.tensor_tensor(out=sq[:], in0=sl[:], in1=sl[:], op=ALU.mult),
                         r=[slk], w=[sqk])
                    ps2, pk2 = psn()
                    S.op("pe", lambda e: e.matmul(ps2[:], ones32[:], sq[:], start=True, stop=True),
                         r=["ones32", sqk], w=[pk2])
                    rn, rnk = t32()
                    S.op("act", lambda e: e.activation(out=rn[:], in_=ps2[:], func=AF.Sqrt, bias=epsrms[:], scale=1.0),
                         r=[pk2, "epsrms"], w=[rnk])
                    S.op("dve", lambda e: e.reciprocal(out=rn[:], in_=rn[:]), r=[rnk], w=[rnk])
                    dst, dkk = (qT, "qkm") if kind == 0 else (kT, "qkm")
                    scl = 128.0 ** -0.5 if kind == 0 else 1.0
                    S.op("dve", lambda e: e.scalar_tensor_tensor(out=dst[:, hh, :], in0=sl[:], scalar=scl, in1=rn[:],
                                                                 op0=ALU.mult, op1=ALU.mult), r=[slk, rnk], w=[dkk])

                fm_tile(pan, (ft % 2) * 128, evac)

            if _st2 == "s4":
                return
            if not is_prefix:
                for p in range(4):
                    wp, wk = load_panel(win[19 + p])
                    for tb in range(4):
                        ps, pk = psn()
                        for k in range(KC):
                            S.op("pe", lambda e, k=k, tb=tb: e.matmul(ps[:, 0:256], uT[:, k, tb * 128:(tb + 1) * 128],
                                                                      wp[:, k, :], start=(k == 0), stop=(k == KC - 1)),
                                 r=[wk, "uT"], w=[pk])
                        S.op("act", lambda e, tb=tb, p=p: e.activation(out=zs[:, tb, p * 256:(p + 1) * 256],
                                                                       in_=ps[:, 0:256], func=AF.Silu),
                             r=[pk], w=["zs"])

            for tb in range(4):
                tc0 = tb * 128
                if not is_prefix:
                    attention_tb(tb, first_main and tb == 0)
                gdn_tb(tb, is_prefix)
            if need_kv:
                S.op("pool", lambda e: e.tensor_copy(out=KT[:, :, 0:128], in_=KT[:, :, 512:640]), r=["KT"], w=["KT"])
                S.op("pool", lambda e: e.tensor_copy(out=VA[:, 0, :, :], in_=VA[:, 4, :, :]), r=["VA"], w=["VA"])
            if is_prefix:
                return
            for i in range(KC):
                gp, gk = load_panel(win[23 + i])
                if i % 2 == 0:
                    pp, ppk = load_panel(wproj[i // 2])
                col = (i % 2) * 128
                psA, pkA = psn()
                for k in range(KC):
                    S.op("pe", lambda e, k=k: e.matmul(psA[:], gp[:, k, 0:128], uT[:, k, :], start=(k == 0),
                                                       stop=(k == KC - 1)), r=[gk, "uT"], w=[pkA])
                sgA, sgAk = t32()
                S.op("act", lambda e: e.activation(out=sgA[:], in_=psA[:], func=AF.Sigmoid), r=[pkA], w=[sgAk])
                psB, pkB = psn()
                for k in range(KC):
                    S.op("pe", lambda e, k=k: e.matmul(psB[:], gp[:, k, 128:256], uT[:, k, :], start=(k == 0),
                                                       stop=(k == KC - 1)), r=[gk, "uT"], w=[pkB])
                sgB, sgBk = t32()
                S.op("act", lambda e: e.activation(out=sgB[:], in_=psB[:], func=AF.Sigmoid), r=[pkB], w=[sgBk])
                psC, pkC = psn()
                for k in range(8):
                    S.op("pe", lambda e, k=k: e.matmul(psC[:], pp[:, k, col:col + 128], oaT[:, k, :], start=(k == 0),
                                                       stop=(k == 7)), r=[ppk, "QT"], w=[pkC])
                S.op("dve", lambda e: e.tensor_tensor(out=sgA[:], in0=sgA[:], in1=psC[:], op=ALU.mult),
                     r=[sgAk, pkC], w=[sgAk])
                psD, pkD = psn()
                for k in range(8):
                    S.op("pe", lambda e, k=k: e.matmul(psD[:], pp[:, 8 + k, col:col + 128], obT[:, k, :],
                                                       start=(k == 0), stop=(k == 7)), r=[ppk, "vT"], w=[pkD])
                S.op("dve", lambda e: e.tensor_tensor(out=sgB[:], in0=sgB[:], in1=psD[:], op=ALU.mult),
                     r=[sgBk, pkD], w=[sgBk])
                S.op("pool", lambda e, i=i: e.tensor_tensor(out=mT[:, i, :], in0=sgA[:], in1=sgB[:], op=ALU.add),
                     r=[sgAk, sgBk], w=["qkm"])
            for half in range(2):
                for j in range(2):
                    tb = half * 2 + j
                    tok0 = sbi_main * 512 + tb * 128
                    S.dma("sp", XT[j][:], xm[tok0:tok0 + 128, :], f"xt{j}", w=[f"xt{j}"])
                for n in range(8):
                    wo, wok = load_panel(wout[n])
                    for j in range(2):
                        tb = half * 2 + j
                        ps, pk = psn()
                        for k in range(KC):
                            S.op("pe", lambda e, k=k, tb=tb: e.matmul(ps[:, 0:PW], mT[:, k, tb * 128:(tb + 1) * 128],
                                                                      wo[:, k, :], start=(k == 0), stop=(k == KC - 1)),
                                 r=[wok, "qkm"], w=[pk])
                        tt, ttk = t32()
                        S.op("dve", lambda e, n=n: e.tensor_tensor(out=tt[:, 0:PW], in0=ps[:, 0:PW],
                                                                   in1=g1bc[:, n * PW:(n + 1) * PW], op=ALU.mult),
                             r=[pk, "g1bc"], w=[ttk])
                        S.op("dve", lambda e, n=n, j=j: e.scalar_tensor_tensor(
                            out=XT[j][:, n * PW:(n + 1) * PW], in0=XT[j][:, n * PW:(n + 1) * PW], scalar=ALPHA,
                            in1=tt[:, 0:PW], op0=ALU.mult, op1=ALU.add), r=[ttk, f"xt{j}"], w=[f"xt{j}"])
                for j in range(2):
                    tb = half * 2 + j
                    tok0 = sbi_main * 512 + tb * 128
                    S.dma("sp", x1s[tok0:tok0 + 128, :], XT[j][:], f"x1st{j}", r=[f"xt{j}"], w=["x1s"])

        def attention_tb(tb, masked_halo):
            tc0 = tb * 128
            for kvh in range(4):
                pt0, pt0k = PT[(kvh % 2) * 2], f"PT{(kvh % 2) * 2}"
                pt1, pt1k = PT[(kvh % 2) * 2 + 1], f"PT{(kvh % 2) * 2 + 1}"
                psA, pkA_ = psn()
                psB, pkB_ = psn()
                for kb, kc0 in ((0, tc0), (1, tc0 + 128)):
                    for half, (ps, pk) in enumerate(((psA, pkA_), (psB, pkB_))):
                        pr = slice(half * 64, half * 64 + 64)
                        S.op("pe", lambda e, ps=ps, pr=pr, kc0=kc0, kb=kb: e.matmul(
                            ps[:, kb * 256:(kb + 1) * 256].rearrange("p (a b) -> p a b", a=2),
                            KT[pr, kvh, kc0:kc0 + 128], QT[pr, 2 * kvh:2 * kvh + 2, tc0:tc0 + 128],
                            start=True, stop=True), r=["KT", "QT"], w=[pk])
                for (ps, pk, sl) in ((psA, pkA_, slice(0, 2)), (psB, pkB_, slice(2, 4))):
                    v3 = ps[:].rearrange("p (kb s q) -> p kb s q", kb=2, s=2)
                    S.op("act", lambda e, v3=v3, sl=sl: e.activation(
                        out=pt0[0:64, sl, 0:64], in_=v3[0:64, 0, :, 0:64], func=AF.Exp,
                        bias=(kbias[0:64] if masked_halo else 0.0), scale=0.125), r=[pk, "cst"], w=[pt0k])
                    S.op("act", lambda e, v3=v3, sl=sl: e.activation(
                        out=pt0[64:128, sl, :], in_=v3[64:128, 0, :, :], func=AF.Exp,
                        bias=(kbias[64:128] if masked_halo else 0.0), scale=0.125), r=[pk, "cst"], w=[pt0k])
                    S.op("act", lambda e, v3=v3, sl=sl: e.activation(
                        out=pt1[0:64, sl, :], in_=v3[0:64, 1, :, :], func=AF.Exp, scale=0.125),
                        r=[pk], w=[pt1k])
                    S.op("act", lambda e, v3=v3, sl=sl: e.activation(
                        out=pt1[64:128, sl, 64:128], in_=v3[64:128, 1, :, 64:128], func=AF.Exp, scale=0.125),
                        r=[pk], w=[pt1k])
                pso, pko = psn()
                for s in range(4):
                    S.op("pe", lambda e, s=s: e.matmul(pso[:, s * 65:(s + 1) * 65], pt0[:, s, :], VA[:, tb, kvh, :],
                                                       start=True, stop=False), r=[pt0k, "VA"], w=[pko])
                    S.op("pe", lambda e, s=s: e.matmul(pso[:, s * 65:(s + 1) * 65], pt1[:, s, :], VA[:, tb + 1, kvh, :],
                                                       start=False, stop=True), r=[pt1k, "VA"], w=[pko])
                S.op("dve", lambda e: e.tensor_copy(out=oaun[:, kvh * 4:(kvh + 1) * 4, :],
                                                    in_=pso[:, 0:260].rearrange("p (s d) -> p s d", s=4)),
                     r=[pko], w=["oaun"])
            den = gsm[:, 64:80]
            S.op("dve", lambda e: e.tensor_tensor(out=den, in0=oaun[:, :, 64], in1=esink, op=ALU.add),
                 r=["oaun", "small"], w=["gsm_den"])
            S.op("dve", lambda e: e.reciprocal(out=den, in_=den), r=["gsm_den"], w=["gsm_den"])
            S.op("dve", lambda e: e.tensor_tensor(out=oa[:].rearrange("p (h d) -> p h d", h=16), in0=oaun[:, :, 0:64],
                                                  in1=den.unsqueeze(2).to_broadcast([128, 16, 64]), op=ALU.mult),
                 r=["oaun", "gsm_den"], w=["oa"])
            psb, pkb = psn()
            psv = psb[:].bitcast(BF16)
            for c in range(8):
                S.op("pe", lambda e, c=c: e.transpose(psv[:, c * 128:(c + 1) * 128], oa[:, c * 128:(c + 1) * 128],
                                                      identb[:]), r=["oa", "identb"], w=[pkb])
            S.op("act", lambda e: e.copy(out=oaT[:, :, tc0:tc0 + 128],
                                         in_=psv[:, 0:1024].rearrange("p (c t) -> p c t", c=8)), r=[pkb], w=["QT"])

        def gdn_tb(tb, is_prefix):
            tc0 = tb * 128
            beta = gsm[:, 0:8]
            g = gsm[:, 8:16]
            cum = gsm[:, 16:24]
            ecum = gsm[:, 24:32]
            ekd = gsm[:, 32:40]
            gm = gsm[:, 40:56]
            decbc = gsm[:, 80:96]
            S.op("act", lambda e: e.activation(out=beta, in_=bd[:, tb, 0:8], func=AF.Sigmoid), r=["bd"], w=["g_beta"])
            tmp = gsm[:, 56:64]
            S.op("dve", lambda e: e.tensor_tensor(out=g, in0=bd[:, tb, 8:16], in1=dtbb, op=ALU.add),
                 r=["bd", "small"], w=["g_g"])
            S.op("dve", lambda e: e.scalar_tensor_tensor(out=tmp, in0=g, scalar=-1.0, in1=g, op0=ALU.mult,
                                                         op1=ALU.max), r=["g_g"], w=["g_tmp"])
            S.op("act", lambda e: e.activation(out=tmp, in_=tmp, func=AF.Exp, scale=-1.0), r=["g_tmp"], w=["g_tmp"])
            S.op("dve", lambda e: e.tensor_scalar(out=tmp, in0=tmp, scalar1=1.0, scalar2=None, op0=ALU.add),
                 r=["g_tmp"], w=["g_tmp"])
            S.op("act", lambda e: e.activation(out=tmp, in_=tmp, func=AF.Ln), r=["g_tmp"], w=["g_tmp"])
            S.op("dve", lambda e: e.scalar_tensor_tensor(out=g, in0=g, scalar=0.0, in1=tmp, op0=ALU.max, op1=ALU.add),
                 r=["g_g", "g_tmp"], w=["g_g"])
            S.op("dve", lambda e: e.tensor_tensor(out=g, in0=g, in1=negA, op=ALU.mult), r=["g_g", "small"], w=["g_g"])
            S.op("dve", lambda e: e.tensor_scalar(out=gm[:, 0:8], in0=g, scalar1=cm0, scalar2=None, op0=ALU.mult),
                 r=["g_g", "cst"], w=["g_gm"])
            S.op("dve", lambda e: e.tensor_scalar(out=gm[:, 8:16], in0=g, scalar1=cm1, scalar2=None, op0=ALU.mult),
                 r=["g_g", "cst"], w=["g_gm"])
            psc, pkc = psn()
            S.op("pe", lambda e: e.matmul(psc[:, 0:8], UT, g, start=True, stop=True), r=["cst", "g_g"], w=[pkc])
            S.op("pe", lambda e: e.matmul(psc[:, 8:16], ONB, g, start=True, stop=True), r=["cst", "g_g"], w=[pkc])
            S.op("pe", lambda e: e.matmul(psc[:, 16:32], ones32[:], gm, start=True, stop=True),
                 r=["ones32", "g_gm"], w=[pkc])
            S.op("dve", lambda e: e.tensor_copy(out=cum, in_=psc[:, 0:8]), r=[pkc], w=["g_cum"])
            S.op("act", lambda e: e.activation(out=ecum, in_=psc[:, 0:8], func=AF.Exp), r=[pkc], w=["g_ecum"])
            S.op("dve", lambda e: e.tensor_tensor(out=ekd, in0=psc[:, 8:16], in1=cum, op=ALU.subtract),
                 r=[pkc, "g_cum"], w=["g_ekd"])
            S.op("act", lambda e: e.activation(out=ekd, in_=ekd, func=AF.Exp), r=["g_ekd"], w=["g_ekd"])
            S.op("act", lambda e: e.activation(out=decbc, in_=psc[:, 16:32], func=AF.Exp), r=[pkc], w=["g_dec"])
            _st3 = _os.environ.get("K_STOP3", "")
            if _st3 == "g1":
                return
            for grp in range(2):
                psb, pkb = psn()
                psv = psb[:].bitcast(BF16)
                for hq in range(4):
                    h = grp * 4 + hq
                    S.op("pe", lambda e, h=h, hq=hq: e.transpose(psv[:, hq * 128:(hq + 1) * 128],
                                                                 kT[:, h, tc0:tc0 + 128], identb[:]),
                         r=["qkm", "identb"], w=[pkb])
                    S.op("pe", lambda e, h=h, hq=hq: e.transpose(psv[:, 512 + hq * 128:512 + (hq + 1) * 128],
                                                                 vT[:, h, tc0:tc0 + 128], identb[:]),
                         r=["vT", "identb"], w=[pkb])
                hs = slice(grp * 4, grp * 4 + 4)
                _sk = _os.environ.get("K_SKIP", "")
                if _sk == "noevac":
                    continue
                kv_ = psv[:, 0:512].rearrange("p (h d) -> p h d", h=4)
                if _sk in ("", "only_kdec"):
                  S.op("dve", lambda e: e.tensor_tensor(out=kdec[:, hs, :], in0=kv_,
                                                      in1=ecum[:, hs].unsqueeze(2).to_broadcast([128, 4, 128]),
                                                      op=ALU.mult), r=[pkb, "g_ecum"], w=["kdec"])
                ekb = gsm[:, 56:64]
                S.op("dve", lambda e: e.tensor_tensor(out=ekb, in0=ekd, in1=beta, op=ALU.mult),
                     r=["g_ekd", "g_beta", "g_tmp"], w=["g_tmp"])
                if _sk in ("", "only_kdd"):
                  S.op("dve", lambda e: e.tensor_tensor(out=kdd[:, hs, :], in0=kv_,
                                                      in1=ekb[:, hs].unsqueeze(2).to_broadcast([128, 4, 128]),
                                                      op=ALU.mult), r=[pkb, "g_tmp"], w=["kdd"])
                if _sk in ("", "only_vtok"):
                  S.op("act", lambda e: e.copy(out=vtok[:, hs, :],
                                             in_=psv[:, 512:1024].rearrange("p (h d) -> p h d", h=4)),
                     r=[pkb], w=["vtok"])
            if _st3 == "g2":
                return
            Pfin = []
            for grp in range(2):
                hs = slice(grp * 4, grp * 4 + 4)
                GU, GUk = t32()
                S.op("dve", lambda e: e.tensor_tensor(
                    out=GU[:].rearrange("p (h c) -> p h c", h=4), in0=UT.unsqueeze(1).to_broadcast([128, 4, 128]),
                    in1=g[:, hs].unsqueeze(2).to_broadcast([128, 4, 128]), op=ALU.mult),
                    r=["cst", "g_g"], w=[GUk])
                psr, pkr = psn()
                S.op("pe", lambda e: e.matmul(psr[:], ONB, GU[:], start=True, stop=True), r=["cst", GUk], w=[pkr])
                E, Ek = t32()
                for hq in range(4):
                    h = grp * 4 + hq
                    S.op("dve", lambda e, h=h, hq=hq: e.scalar_tensor_tensor(
                        out=E[:, hq * 128:(hq + 1) * 128], in0=psr[:, hq * 128:(hq + 1) * 128],
                        scalar=cum[:, h:h + 1], in1=NEGM, op0=ALU.subtract, op1=ALU.min),
                        r=[pkr, "g_cum", "cst"], w=[Ek])
                S.op("act", lambda e: e.activation(out=E[:], in_=E[:], func=AF.Exp), r=[Ek], w=[Ek])
                E3 = E[:].rearrange("p (h c) -> p h c", h=4)
                S.op("dve", lambda e: e.tensor_tensor(out=E3, in0=E3,
                                                      in1=beta[:, hs].unsqueeze(2).to_broadcast([128, 4, 128]),
                                                      op=ALU.mult), r=[Ek, "g_beta"], w=[Ek])
                if not is_prefix:
                    psq, pkq = psn()
                    for hq in range(4):
                        h = grp * 4 + hq
                        S.op("pe", lambda e, h=h, hq=hq: e.matmul(psq[:, hq * 128:(hq + 1) * 128],
                                                                  kT[:, h, tc0:tc0 + 128], qT[:, h, tc0:tc0 + 128],
                                                                  start=True, stop=True), r=["qkm", "qkm"], w=[pkq])
                    S.op("dve", lambda e: e.tensor_tensor(out=QKm[grp][:].rearrange("p h c -> p (h c)"), in0=psq[:],
                                                          in1=E[:], op=ALU.mult), r=[pkq, Ek], w=[f"QKm{grp}"])
                psk, pkk = psn()
                for hq in range(4):
                    h = grp * 4 + hq
                    S.op("pe", lambda e, h=h, hq=hq: e.matmul(psk[:, hq * 128:(hq + 1) * 128],
                                                              kT[:, h, tc0:tc0 + 128], kT[:, h, tc0:tc0 + 128],
                                                              start=True, stop=True), r=["qkm"], w=[pkk])
                S.op("pool", lambda e: e.tensor_tensor(out=E3, in0=E3, in1=SM.unsqueeze(1).to_broadcast([128, 4, 128]),
                                                       op=ALU.mult), r=[Ek, "cst"], w=[Ek])
                xi = grp * 2
                X, Xk = Xb[xi], f"Xb{xi}"
                S.op("dve", lambda e: e.tensor_tensor(out=X[:].rearrange("p h c -> p (h c)"), in0=psk[:], in1=E[:],
                                                      op=ALU.mult), r=[pkk, Ek], w=[Xk])
                pst, pkt = psn()
                ptv = pst[:].bitcast(BF16)
                for hq in range(4):
                    S.op("pe", lambda e, hq=hq: e.transpose(ptv[:, hq * 128:(hq + 1) * 128], X[:, hq, :], identb[:]),
                         r=[Xk, "identb"], w=[pkt])
                Y, Yk = Yb[xi], f"Yb{xi}"
                S.op("act", lambda e: e.copy(out=Y[:].rearrange("p h c -> p (h c)"), in_=ptv[:, 0:512]),
                     r=[pkt], w=[Yk])
                P_, Pk = Pb[xi], f"Pb{xi}"
                S.op("pool", lambda e: e.tensor_tensor(out=P_[:], in0=identb[:].unsqueeze(1).to_broadcast([128, 4, 128]),
                                                       in1=X[:], op=ALU.subtract), r=["identb", Xk], w=[Pk])
                for m in range(1, 6):
                    nx = xi + (m % 2)
                    cx = xi + ((m + 1) % 2)
                    Xc, Xck, Yc, Yck = Xb[cx], f"Xb{cx}", Yb[cx], f"Yb{cx}"
                    Xn, Xnk, Yn, Ynk = Xb[nx], f"Xb{nx}", Yb[nx], f"Yb{nx}"
                    psy, pky = psn()
                    for hq in range(4):
                        S.op("pe", lambda e, hq=hq: e.matmul(psy[:, hq * 128:(hq + 1) * 128], Xc[:, hq, :],
                                                             Yc[:, hq, :], start=True, stop=True),
                             r=[Xck, Yck], w=[pky])
                    if m < 5:
                        psx, pkx = psn()
                        for hq in range(4):
                            S.op("pe", lambda e, hq=hq: e.matmul(psx[:, hq * 128:(hq + 1) * 128], Yc[:, hq, :],
                                                                 Xc[:, hq, :], start=True, stop=True),
                                 r=[Xck, Yck], w=[pkx])
                    S.op("act", lambda e: e.copy(out=Yn[:].rearrange("p h c -> p (h c)"), in_=psy[:]),
                         r=[pky], w=[Ynk])
                    if m < 5:
                        S.op("dve", lambda e: e.tensor_copy(out=Xn[:].rearrange("p h c -> p (h c)"), in_=psx[:]),
                             r=[pkx], w=[Xnk])
                    Pc, Pck = Pb[cx], f"Pb{cx}"
                    Pn, Pnk = Pb[nx], f"Pb{nx}"
                    psp, pkp = psn()
                    for hq in range(4):
                        S.op("pe", lambda e, hq=hq: e.matmul(psp[:, hq * 128:(hq + 1) * 128], Yn[:, hq, :],
                                                             Pc[:, hq, :], start=True, stop=True),
                             r=[Ynk, Pck], w=[pkp])
                    S.op("dve", lambda e: e.tensor_tensor(out=Pn[:].rearrange("p h c -> p (h c)"), in0=psp[:],
                                                          in1=Pc[:].rearrange("p h c -> p (h c)"), op=ALU.add),
                         r=[pkp, Pck], w=[Pnk])
                Pfin.append((Pb[xi + 1], f"Pb{xi + 1}"))
            if _st3 == "g3":
                return
            for grp in range(2):
                P_, Pk = Pfin[grp]
                psu, pku = psn()
                psw, pkw = psn()
                for hq in range(4):
                    h = grp * 4 + hq
                    S.op("pe", lambda e, h=h, hq=hq: e.matmul(psu[:, hq * 128:(hq + 1) * 128], P_[:, hq, :],
                                                              vtok[:, h, :], start=True, stop=True),
                         r=[Pk, "vtok"], w=[pku])
                    S.op("pe", lambda e, h=h, hq=hq: e.matmul(psw[:, hq * 128:(hq + 1) * 128], kdec[:, h, :],
                                                              P_[:, hq, :], start=True, stop=True),
                         r=[Pk, "kdec"], w=[pkw])
                hs = slice(grp * 4, grp * 4 + 4)
                S.op("act", lambda e: e.copy(out=up[:, hs, :].rearrange("p h c -> p (h c)"), in_=psu[:]),
                     r=[pku], w=["up"])
                S.op("act", lambda e: e.mul(out=nwT[:, hs, :].rearrange("p h c -> p (h c)"), in_=psw[:], mul=-1.0),
                     r=[pkw], w=["nwT"])
            if _st3 == "g4":
                return
            for ci in range(2):
                pr = slice(ci * 64, ci * 64 + 64)
                cur = sbi[0]
                Sc, Sck = Sb[cur], f"Sb{cur}"
                Sn, Snk = Sb[cur ^ 1], f"Sb{cur ^ 1}"
                pws = []
                for grp in range(2):
                    psw, pkw = psn()
                    pws.append((psw, pkw))
                    for hq in range(4):
                        h = grp * 4 + hq
                        S.op("pe", lambda e, h=h, hq=hq: e.matmul(psw[:, hq * 128:(hq + 1) * 128], nwT[:, h, :],
                                                                  Sc[:, h, :], start=True, stop=True),
                             r=["nwT", Sck], w=[pkw])
                for grp in range(2):
                    psw, pkw = pws[grp]
                    hs = slice(grp * 4, grp * 4 + 4)
                    S.op("dve", lambda e: e.tensor_tensor(out=vn[pr, hs, :].rearrange("p h c -> p (h c)"),
                                                          in0=psw[pr, :],
                                                          in1=up[pr, hs, :].rearrange("p h c -> p (h c)"), op=ALU.add),
                         r=[pkw, "up"], w=["vn"])
                if not is_prefix:
                    for grp in range(2):
                        pso, pko = psn()
                        hs = slice(grp * 4, grp * 4 + 4)
                        for hq in range(4):
                            h = grp * 4 + hq
                            S.op("pe", lambda e, h=h, hq=hq: e.matmul(pso[:, hq * 128:(hq + 1) * 128],
                                                                      qT[:, h, tc0:tc0 + 128], Sc[:, h, :],
                                                                      start=True, stop=True), r=["qkm", Sck], w=[pko])
                        S.op("dve", lambda e: e.tensor_tensor(
                            out=oacc[pr, hs, :], in0=pso[pr, :].rearrange("p (h c) -> p h c", h=4),
                            in1=ecum[pr, hs].unsqueeze(2).to_broadcast([64, 4, 128]), op=ALU.mult),
                            r=[pko, "g_ecum"], w=["oacc"])
                pus = []
                for grp in range(2):
                    psu, pku = psn()
                    pus.append((psu, pku))
                    for hq in range(4):
                        h = grp * 4 + hq
                        S.op("pe", lambda e, h=h, hq=hq: e.matmul(psu[:, hq * 128:(hq + 1) * 128], kdd[pr, h, :],
                                                                  vn[pr, h, :], start=True, stop=True),
                             r=["kdd", "vn"], w=[pku])
                for grp in range(2):
                    psu, pku = pus[grp]
                    hs = slice(grp * 4, grp * 4 + 4)
                    S.op("dve", lambda e: e.tensor_tensor(
                        out=S32[:, hs, :], in0=S32[:, hs, :],
                        in1=decbc[:, ci * 8 + grp * 4:ci * 8 + grp * 4 + 4].unsqueeze(2).to_broadcast([128, 4, 128]),
                        op=ALU.mult), r=["S32", "g_dec"], w=["S32"])
                    S.op("dve", lambda e: e.tensor_tensor(out=S32[:, hs, :].rearrange("p h c -> p (h c)"),
                                                          in0=S32[:, hs, :].rearrange("p h c -> p (h c)"),
                                                          in1=psu[:], op=ALU.add), r=["S32", pku], w=["S32"])
                    S.op("act", lambda e: e.copy(out=Sn[:, hs, :], in_=S32[:, hs, :]), r=["S32"], w=[Snk])
                sbi[0] ^= 1
            if is_prefix:
                return
            for grp in range(2):
                hs = slice(grp * 4, grp * 4 + 4)
                pso, pko = psn()
                for hq in range(4):
                    h = grp * 4 + hq
                    S.op("pe", lambda e, h=h, hq=hq: e.matmul(pso[:, hq * 128:(hq + 1) * 128], QKm[grp][:, hq, :],
                                                              vn[:, h, :], start=True, stop=True),
                         r=[f"QKm{grp}", "vn"], w=[pko])
                S.op("dve", lambda e: e.tensor_tensor(out=oacc[:, hs, :].rearrange("p h c -> p (h c)"),
                                                      in0=oacc[:, hs, :].rearrange("p h c -> p (h c)"), in1=pso[:],
                                                      op=ALU.add), r=["oacc", pko], w=["oacc"])
            sq, sqk = t32()
            sq2, sq2k = t32()
            ms = gsm[:, 56:64]
            for grp, (sqq, sqqk) in enumerate(((sq, sqk), (sq2, sq2k))):
                hs = slice(grp * 4, grp * 4 + 4)
                S.op("pool", lambda e, sqq=sqq: e.tensor_tensor(out=sqq[:].rearrange("p (h c) -> p h c", h=4),
                                                                in0=oacc[:, hs, :], in1=oacc[:, hs, :], op=ALU.mult),
                     r=["oacc"], w=[sqqk])
                S.op("dve", lambda e, sqq=sqq: e.reduce_sum(out=ms[:, hs], in_=sqq[:].rearrange("p (h c) -> p h c", h=4),
                                                            axis=AX.X), r=[sqqk, "g_tmp"], w=["g_tmp"])
            S.op("act", lambda e: e.activation(out=ms, in_=ms, func=AF.Sqrt, bias=epsrms[:], scale=1.0 / 128.0),
                 r=["g_tmp", "epsrms"], w=["g_tmp"])
            S.op("dve", lambda e: e.reciprocal(out=ms, in_=ms), r=["g_tmp"], w=["g_tmp"])
            S.op("pool", lambda e: e.tensor_tensor(out=zs[:, tb, :].rearrange("p (h c) -> p h c", h=8),
                                                   in0=zs[:, tb, :].rearrange("p (h c) -> p h c", h=8),
                                                   in1=normwb.unsqueeze(1).to_broadcast([128, 8, 128]), op=ALU.mult),
                 r=["zs", "small"], w=["zs"])
            S.op("dve", lambda e: e.tensor_tensor(out=oacc[:], in0=oacc[:],
                                                  in1=ms.unsqueeze(2).to_broadcast([128, 8, 128]), op=ALU.mult),
                 r=["oacc", "g_tmp"], w=["oacc"])
            S.op("dve", lambda e: e.tensor_tensor(out=ob[:].rearrange("p (h c) -> p h c", h=8), in0=oacc[:],
                                                  in1=zs[:, tb, :].rearrange("p (h c) -> p h c", h=8), op=ALU.mult),
                 r=["oacc", "zs"], w=["ob"])
            psb, pkb = psn()
            psv = psb[:].bitcast(BF16)
            for c in range(8):
                S.op("pe", lambda e, c=c: e.transpose(psv[:, c * 128:(c + 1) * 128], ob[:, c * 128:(c + 1) * 128],
                                                      identb[:]), r=["ob", "identb"], w=[pkb])
            S.op("act", lambda e: e.copy(out=obT[:, :, tc0:tc0 + 128],
                                         in_=psv[:, 0:1024].rearrange("p (c t) -> p c t", c=8)), r=[pkb], w=["vT"])

        for s in range(NSBP):
            if _stop in ("p0",):
                break
            mixer_sb(xp[s * 512:(s + 1) * 512, :], True, s == NSBP - 1, False, -1)
        for s in range(NSBM):
            if _stop in ("p0", "pp"):
                break
            mixer_sb(xm[s * 512:(s + 1) * 512, :], False, True, s == 0, s)

        S.barrier()
        es1.close()
        es15 = contextlib.ExitStack()

        def sb(name, shape, dt=F32):
            return es15.enter_context(nc.sbuf_tensor(name, list(shape), dt))

        lnb1 = sb("lnb1", [128, 2, D])
        for i in range(2):
            S.dma("sp", lnb1[:, i, :], lnv[i].partition_broadcast(128), "lnb1", w=["lnb1"])
        for tbi in range(NTB):
            if _stop in ("p0", "pp", "p1"):
                break
            xa = XT[xti[0]]
            xk = f"xt{xti[0]}"
            xti[0] ^= 1
            S.dma("sp", xa[:], x1s[tbi * 128:(tbi + 1) * 128, :], xk, r=["x1s"], w=[xk])
            ln_stats(xa[:], xk)
            ln_normalize(xa[:], xk, xa[:], xk)
            S.op("dve", lambda e: e.tensor_tensor(out=xa[:], in0=xa[:], in1=lnb1[:, 0, :], op=ALU.mult),
                 r=[xk, "lnb1"], w=[xk])
            S.op("pool", lambda e: e.tensor_tensor(out=xa[:], in0=xa[:], in1=lnb1[:, 1, :], op=ALU.add),
                 r=[xk, "lnb1"], w=[xk])
            S.dma("sp", x1s[tbi * 128:(tbi + 1) * 128, :], xa[:], f"x1st{xk}", r=[xk], w=["x1s"])
        S.barrier()
        es15.close()
        es2 = contextlib.ExitStack()

        def sb(name, shape, dt=F32):
            return es2.enter_context(nc.sbuf_tensor(name, list(shape), dt))

        GT = TOKM // NG
        GTB = GT // 128
        g2bc = sb("g2bc", [128, D])
        S.dma("sp", g2bc[:], gscr[1], "g2bc", r=["gscr"], w=["g2bc"])
        u2T = sb("u2T", [128, KC, GT], BF16)
        u32 = sb("u32", [128, KC, 128])
        yacc = sb("yacc", [128, GTB, D])
        comb = sb("comb", [128, GTB, NEXP])
        lnb = sb("lnb", [128, 2, D])
        for i in range(2):
            S.dma("sp", lnb[:, i, :], lnv[2 + i].partition_broadcast(128), "lnb", w=["lnb"])
        actT = sb("actT", [128, 4, GT], BF16)
        rt = sb("rt", [128, 128])

        def load_x1(tok0, xa, xk):
            S.dma("sp", xa[:], x1s[tok0:tok0 + 128, :], xk, r=["x1s"], w=[xk])

        for gI in range(NG):
            if _stop in ("p0", "pp", "p1", "p15"):
                break
            for tb in range(GTB):
                tok0 = gI * GT + tb * 128
                xa = XT[xti[0]]
                xk = f"xt{xti[0]}"
                xti[0] ^= 1
                load_x1(tok0, xa, xk)
                ln_stats(xa[:], xk)
                ln_normalize(xa[:], xk, xa[:], xk)
                mod_transpose(xa[:], xk, sc2, sh2, lambda k, tb=tb: u2T[:, k, tb * 128:(tb + 1) * 128], "u2T",
                              dst32=lambda k: u32[:, k, :], d32k="u32")
                psr, pkr = psn()
                for k in range(KC):
                    S.op("pe", lambda e, k=k: e.matmul(psr[:, 0:36], u32[:, k, :], wr_sb[:, k, :], start=(k == 0),
                                                       stop=(k == KC - 1)), r=["u32", "wr"], w=[pkr])
                lg = rt[:, 0:36]
                S.op("dve", lambda e: e.tensor_tensor(out=lg, in0=psr[:, 0:36], in1=brb, op=ALU.add),
                     r=[pkr, "small"], w=["rt"])
                gmax = rt[:, 36:37]
                S.op("dve", lambda e: e.reduce_max(out=gmax, in_=rt[:, 0:4], axis=AX.X), r=["rt"], w=["rt"])
                oh = rt[:, 40:44]
                S.op("dve", lambda e: e.tensor_scalar(out=oh, in0=rt[:, 0:4], scalar1=gmax, scalar2=None,
                                                      op0=ALU.is_ge), r=["rt"], w=["rt"])
                ge = rt[:, 44:48]
                S.op("dve", lambda e: e.tensor_scalar(out=ge, in0=rt[:, 0:4], scalar1=gmax, scalar2=None,
                                                      op0=ALU.subtract), r=["rt"], w=["rt"])
                gs = rt[:, 37:38]
                S.op("act", lambda e: e.activation(out=ge, in_=ge, func=AF.Exp, accum_out=gs), r=["rt"], w=["rt"])
                el = rt[:, 48:56]
                msk = rt[:, 56:88]
                S.op("dve", lambda e: e.tensor_tensor(out=msk.rearrange("p (g x) -> p g x", g=4),
                                                      in0=rt[:, 4:36].rearrange("p (g x) -> p g x", g=4),
                                                      in1=oh.unsqueeze(2).to_broadcast([128, 4, 8]), op=ALU.mult),
                     r=["rt"], w=["rt"])
                S.op("dve", lambda e: e.reduce_sum(out=el, in_=msk.rearrange("p (g x) -> p x g", g=4), axis=AX.X),
                     r=["rt"], w=["rt"])
                top8 = rt[:, 88:96]
                S.op("dve", lambda e: e.max(out=top8, in_=el), r=["rt"], w=["rt"])
                ee = rt[:, 96:104]
                S.op("dve", lambda e: e.tensor_scalar(out=ee, in0=el, scalar1=top8[:, 0:1], scalar2=None,
                                                      op0=ALU.subtract), r=["rt"], w=["rt"])
                S.op("act", lambda e: e.activation(out=ee, in_=ee, func=AF.Exp), r=["rt"], w=["rt"])
                m2 = rt[:, 104:112]
                S.op("dve", lambda e: e.tensor_scalar(out=m2, in0=el, scalar1=top8[:, 1:2], scalar2=None,
                                                      op0=ALU.is_ge), r=["rt"], w=["rt"])
                S.op("dve", lambda e: e.tensor_tensor(out=ee, in0=ee, in1=m2, op=ALU.mult), r=["rt"], w=["rt"])
                den = rt[:, 38:39]
                S.op("dve", lambda e: e.reduce_sum(out=den, in_=ee, axis=AX.X), r=["rt"], w=["rt"])
                S.op("dve", lambda e: e.tensor_tensor(out=den, in0=den, in1=gs, op=ALU.mult), r=["rt"], w=["rt"])
                S.op("dve", lambda e: e.reciprocal(out=den, in_=den), r=["rt"], w=["rt"])
                S.op("dve", lambda e: e.tensor_scalar(out=ee, in0=ee, scalar1=den, scalar2=None, op0=ALU.mult),
                     r=["rt"], w=["rt"])
                S.op("dve", lambda e, tb=tb: e.tensor_tensor(
                    out=comb[:, tb, :].rearrange("p (g x) -> p g x", g=4),
                    in0=oh.unsqueeze(2).to_broadcast([128, 4, 8]), in1=ee.unsqueeze(1).to_broadcast([128, 4, 8]),
                    op=ALU.mult), r=["rt"], w=["comb"])
            S.op("pool", lambda e: e.memset(yacc[:], 0.0), w=["yacc"])
            for ex in range(NEXP):
                for j in range(4):
                    wp, wk = load_panel(wexp[ex, j])
                    for sbk in range(GT // 512):
                        tk0 = sbk * 512
                        psg, pkg = psn()
                        for k in range(KC):
                            S.op("pe", lambda e, k=k: e.matmul(psg[:], wp[:, k, 0:128], u2T[:, k, tk0:tk0 + 512],
                                                               start=(k == 0), stop=(k == KC - 1)),
                                 r=[wk, "u2T"], w=[pkg])
                        psu_, pku_ = psn()
                        for k in range(KC):
                            S.op("pe", lambda e, k=k: e.matmul(psu_[:], wp[:, k, 128:256], u2T[:, k, tk0:tk0 + 512],
                                                               start=(k == 0), stop=(k == KC - 1)),
                                 r=[wk, "u2T"], w=[pku_])
                        sg, sgk = t32()
                        S.op("act", lambda e: e.activation(out=sg[:], in_=psg[:], func=AF.Silu), r=[pkg], w=[sgk])
                        S.op("dve", lambda e, j=j: e.tensor_tensor(out=actT[:, j, tk0:tk0 + 512], in0=sg[:],
                                                                   in1=psu_[:], op=ALU.mult),
                             r=[sgk, pku_], w=["actT"])
                for dh in range(2):
                    wd, wdk = load_panel(wexp[ex, 4 + dh])
                    for tb in range(GTB):
                        for dq in range(2):
                            psy, pky = psn()
                            for j in range(4):
                                S.op("pe", lambda e, j=j, dq=dq, tb=tb: e.matmul(
                                    psy[:].rearrange("p (a b) -> p a b", a=2),
                                    actT[:, j, tb * 128:(tb + 1) * 128],
                                    wd[:, j * 4 + dq * 2:j * 4 + dq * 2 + 2, :], start=(j == 0), stop=(j == 3)),
                                    r=[wdk, "actT"], w=[pky])
                            c0 = dh * 1024 + dq * 512
                            S.op("dve", lambda e, tb=tb, c0=c0: e.scalar_tensor_tensor(
                                out=yacc[:, tb, c0:c0 + 512], in0=psy[:], scalar=comb[:, tb, ex:ex + 1],
                                in1=yacc[:, tb, c0:c0 + 512], op0=ALU.mult, op1=ALU.add),
                                r=[pky, "comb", "yacc"], w=["yacc"])
            for tb in range(GTB):
                tok0 = gI * GT + tb * 128
                xa = XT[xti[0]]
                xk = f"xt{xti[0]}"
                xti[0] ^= 1
                load_x1(tok0, xa, xk)
                S.op("dve", lambda e, tb=tb: e.tensor_tensor(out=yacc[:, tb, :], in0=yacc[:, tb, :], in1=g2bc[:],
                                                             op=ALU.mult), r=["yacc", "g2bc"], w=["yacc"])
                S.op("dve", lambda e, tb=tb: e.scalar_tensor_tensor(out=xa[:], in0=xa[:], scalar=ALPHA,
                                                                    in1=yacc[:, tb, :], op0=ALU.mult, op1=ALU.add),
                     r=[xk, "yacc"], w=[xk])
                ln_stats(xa[:], xk)
                ln_normalize(xa[:], xk, xa[:], xk)
                S.op("dve", lambda e: e.tensor_tensor(out=xa[:], in0=xa[:], in1=lnb[:, 0, :], op=ALU.mult),
                     r=[xk, "lnb"], w=[xk])
                S.op("pool", lambda e: e.tensor_tensor(out=xa[:], in0=xa[:], in1=lnb[:, 1, :], op=ALU.add),
                     r=[xk, "lnb"], w=[xk])
                S.dma("sp", out[tok0:tok0 + 128, :], xa[:], f"ost{xk}", r=[xk], w=["out"])
        for slot, (s, sid, val) in S.slots.items():
            nc.sync.wait_ge(s, val)
        DEBUG["ninst"] = S.ninst
        es2.close()
    return nc, list(dbg_out)


def _panelize(w, ncols_total=None):
    K, C = w.shape
    assert K == D and C % PW == 0
    return np.ascontiguousarray(w.reshape(KC, 128, C // PW, PW).transpose(2, 1, 0, 3))


def _consts(flag):
    c = np.zeros((128, 5 * 128 + 4), np.float32)
    t = np.arange(128)
    same = (t[:, None] // 64) == (t[None, :] // 64)
    c[:, 0:128] = np.eye(128)
    c[:, 128:256] = (same & (t[:, None] <= t[None, :]))
    c[:, 256:384] = same
    c[:, 384:512] = np.where(same & (t[None, :] >= t[:, None]), 0.0, NEG)
    c[:, 512:640] = (same & (t[None, :] > t[:, None]))
    c[:, 640] = flag
    c[:, 641] = (flag - 1.0) * 30000.0
    c[:64, 642] = 1.0
    c[64:, 643] = 1.0
    return c


def prepare(inputs, n_cores=8):
    f = lambda a: np.asarray(a, dtype=np.float32)
    x = f(inputs["x"])
    B, SEQ, _ = x.shape
    HALF = SEQ // 2
    w_in = f(inputs["w_in"])[0]
    qperm = np.zeros(1024, np.int64)
    for kvh in range(4):
        for cc in range(2):
            for half in range(2):
                dst = (2 * kvh + cc) * 128 + half * 64
                src = (4 * kvh + 2 * half + cc) * 64
                qperm[dst:dst + 64] = np.arange(src, src + 64)
    kdup = np.concatenate([np.concatenate([np.arange(1024 + h * 64, 1024 + h * 64 + 64)] * 2) for h in range(4)])
    gcols = np.concatenate([np.concatenate([np.arange(5648 + i * 128, 5648 + (i + 1) * 128),
                                            np.arange(7696 + i * 128, 7696 + (i + 1) * 128)]) for i in range(16)])
    cols = np.concatenate([qperm, kdup, np.arange(1280, 1536), np.arange(1536, 5632), gcols])
    win = _panelize(w_in[:, cols])
    wbd = np.ascontiguousarray(w_in[:, 5632:5648].reshape(KC, 128, 16).transpose(1, 0, 2))
    wada = _panelize(f(inputs["w_ada"])[0])
    b_ada = f(inputs["b_ada"])[0]
    badaT = np.ascontiguousarray(b_ada.reshape(96, 128).T)
    badag = np.ascontiguousarray(np.stack([b_ada[4096:6144], b_ada[10240:12288]]))
    convw = np.ascontiguousarray(f(inputs["conv_w"])[0].reshape(4, 24, 128).transpose(2, 1, 0))
    wpa = f(inputs["w_proj_a"])[0]
    wpb = f(inputs["w_proj_b"])[0]
    wproj = np.ascontiguousarray(np.concatenate([wpa, wpb], 0).reshape(KC, 128, 8, PW).transpose(2, 1, 0, 3))
    wout = _panelize(f(inputs["w_out"])[0])
    lnv = np.ascontiguousarray(np.stack([f(inputs["ln1_g"])[0], f(inputs["ln1_b"])[0], f(inputs["ln2_g"])[0],
                                         f(inputs["ln2_b"])[0]]))
    wrt = np.concatenate([f(inputs["w_router_group"])[0], f(inputs["w_router_expert"])[0]], 1)
    wr = np.ascontiguousarray(wrt.reshape(KC, 128, 36).transpose(1, 0, 2))
    br = np.concatenate([f(inputs["b_router_group"])[0], f(inputs["b_router_expert"])[0]])
    wgu = f(inputs["w_gate_up"])[0]
    wdn = f(inputs["w_down"])[0]
    wexp = np.empty((NEXP, 6, 128, KC, PW), np.float32)
    gu = wgu.reshape(NEXP, KC, 128, 2, 4, 128)
    wexp[:, 0:4] = gu.transpose(0, 4, 2, 1, 3, 5).reshape(NEXP, 4, 128, KC, PW)
    dn = wdn.reshape(NEXP, 4, 128, 2, 4, PW)
    wexp[:, 4:6] = dn.transpose(0, 3, 2, 1, 4, 5).reshape(NEXP, 2, 128, KC, PW)
    shared = dict(wada=wada, badaT=badaT, badag=badag, win=win, wbd=wbd, convw=convw,
                  sinks=f(inputs["swa_sinks"])[0], alog=f(inputs["gdn_a_log"])[0], dtb=f(inputs["gdn_dt_bias"])[0],
                  normw=f(inputs["gdn_norm_w"])[0], wproj=wproj, wout=wout, lnv=lnv, wr=wr, br=br, wexp=wexp)
    c = f(inputs["c"])
    in_maps = []
    for core in range(n_cores):
        b, h = core // 2, core % 2
        m = dict(shared)
        m["xm"] = np.ascontiguousarray(x[b, h * HALF:(h + 1) * HALF])
        m["xp"] = np.ascontiguousarray(x[b, 0:HALF]) if h == 1 else np.zeros((HALF, D), np.float32)
        m["cT"] = np.ascontiguousarray(c[b].reshape(KC, 128).T)
        m["consts"] = _consts(float(h))
        in_maps.append(m)
    return in_maps, (B, SEQ, HALF)


_CACHE = {}


def kernel(**inputs):
    in_maps, (B, SEQ, HALF) = prepare(inputs)
    nsb = HALF // 512
    key = (nsb,)
    if key not in _CACHE:
        _CACHE[key] = build_nc(nsb, nsb, 2 if nsb >= 4 else 1)[0]
    nc = _CACHE[key]
    res = run_bass_kernel_spmd(nc, in_maps, core_ids=list(range(8)))
    outp = np.empty((B, SEQ, D), np.float32)
    for core in range(8):
        b, h = core // 2, core % 2
        outp[b, h * HALF:(h + 1) * HALF] = res.results[core]["out"]
    return outp
```

```python
import contextlib
import numpy as np
import concourse.bass as bass
import concourse.mybir as mybir
from concourse.bass_utils import run_bass_kernel_spmd

F32 = mybir.dt.float32
BF16 = mybir.dt.bfloat16
AF = mybir.ActivationFunctionType
ALU = mybir.AluOpType
AX = mybir.AxisListType

D = 2048
KC = 16
NEXP = 32
ALPHA = 2.0 ** 0.25
LN_EPS = 1e-5
RMS_EPS = 1e-6
NEG = -30000.0
PW = 256
DEBUG = {}


class Sched:
    def __init__(self, nc, es):
        self.nc = nc
        self.es = es
        self.eng = {"pe": nc.tensor, "act": nc.scalar, "dve": nc.vector, "pool": nc.gpsimd, "sp": nc.sync}
        self.cnt = {}
        self.sem = {}
        self.nsem = 0
        self.lastw = {}
        self.readers = {}
        self.seen = {e: {} for e in self.eng}
        self.slots = {}
        self.ninst = 0
        for e in ("pe", "act", "dve", "pool"):
            self._newsem(e)

    def _newsem(self, e):
        self.nsem += 1
        s = self.es.enter_context(self.nc.semaphore(f"s_{e}_{self.nsem}"))
        self.sem[e] = (s, self.nsem)
        self.cnt[e] = 0

    def _wait(self, e, deps):
        best = {}
        for (s, sid, val, se) in deps:
            if se == e and e == "pe":
                continue
            if best.get(sid, (None, 0))[1] < val:
                best[sid] = (s, val)
        seen = self.seen[e]
        for sid, (s, val) in best.items():
            if seen.get(sid, 0) >= val:
                continue
            self.eng[e].wait_ge(s, val)
            seen[sid] = val

    def _collect(self, r, w, e=None):
        deps = []
        for k in r:
            x = self.lastw.get(k)
            if x is not None:
                deps.append(x)
            if k.startswith("ps"):
                rd = self.readers.get(k)
                if rd:
                    deps.extend(v for v in rd.values() if v[3] != e)
        for k in w:
            x = self.lastw.get(k)
            if x is not None:
                deps.append(x)
            rd = self.readers.get(k)
            if rd:
                deps.extend(rd.values())
        return deps

    def _record(self, ev, r, w):
        for k in r:
            d = self.readers.setdefault(k, {})
            d[ev[1]] = ev
        for k in w:
            self.lastw[k] = ev
            self.readers[k] = {}

    def op(self, e, fn, r=(), w=()):
        self._wait(e, self._collect(r, w, e))
        ins = fn(self.eng[e])
        if self.cnt[e] >= 40000:
            self._newsem(e)
        s, sid = self.sem[e]
        self.cnt[e] += 1
        ins.then_inc(s, 1)
        ev = (s, sid, self.cnt[e], e)
        self._record(ev, r, w)
        self.ninst += 1
        return ev

    def dma(self, e, out, in_, slot, r=(), w=()):
        self._wait(e, self._collect(r, w, e))
        if slot not in self.slots:
            self.nsem += 1
            s = self.es.enter_context(self.nc.semaphore(f"d_{self.nsem}"))
            self.slots[slot] = [s, self.nsem, 0]
        sl = self.slots[slot]
        self.eng[e].dma_start(out=out, in_=in_).then_inc(sl[0], 16)
        sl[2] += 16
        ev = (sl[0], sl[1], sl[2], "dma")
        self._record(ev, r, w)
        self.ninst += 1
        return ev

    def barrier(self):
        evs = []
        for e in ("pe", "act", "dve", "pool"):
            s, sid = self.sem[e]
            if self.cnt[e] > 0:
                evs.append((s, sid, self.cnt[e], e))
        for slot, (s, sid, val) in self.slots.items():
            if val > 0:
                evs.append((s, sid, val, "dma"))
        for e in self.eng:
            self._wait(e, evs)
        self.lastw.clear()
        self.readers.clear()


def build_nc(NSBP, NSBM, NG, dbg=False):
    TOKM = NSBM * 512
    TOKP = NSBP * 512
    NTB = TOKM // 128
    nc = bass.Bass("TRN2", target_bir_lowering=False)

    def din(name, shape, dt=F32):
        return nc.dram_tensor(name, list(shape), dt, kind="ExternalInput").ap()

    xm = din("xm", [TOKM, D])
    xp = din("xp", [TOKP, D])
    cT = din("cT", [128, KC])
    wada = din("wada", [48, 128, KC, PW])
    badaT = din("badaT", [128, 96])
    badag = din("badag", [2, D])
    win = din("win", [39, 128, KC, PW])
    wbd = din("wbd", [128, KC, 16])
    convw = din("convw", [128, 24, 4])
    sinks = din("sinks", [16])
    alog = din("alog", [8])
    dtb = din("dtb", [8])
    normw = din("normw", [128])
    wproj = din("wproj", [8, 128, KC, PW])
    wout = din("wout", [8, 128, KC, PW])
    lnv = din("lnv", [4, D])
    wr = din("wr", [128, KC, 36])
    br = din("br", [36])
    wexp = din("wexp", [NEXP, 6, 128, KC, PW])
    consts = din("consts", [128, 5 * 128 + 4])
    x1s = nc.dram_tensor("x1s", [TOKM, D], F32, kind="Internal").ap()
    gscr = nc.dram_tensor("gscr", [2, 128, D], F32, kind="Internal").ap()
    out = nc.dram_tensor("out", [TOKM, D], F32, kind="ExternalOutput").ap()
    dbg_out = {}

    with contextlib.ExitStack() as es:
        S = Sched(nc, es)

        def sb(name, shape, dt=F32):
            return es.enter_context(nc.sbuf_tensor(name, list(shape), dt))

        PS = [es.enter_context(nc.psum_tensor(f"ps{i}", [128, 512], F32)) for i in range(8)]
        psi = [0]

        def psn():
            i = psi[0]
            psi[0] = (i + 1) % 8
            return PS[i], f"ps{i}"

        NT32 = 6
        T32 = [sb(f"t32_{i}", [128, 512], F32) for i in range(NT32)]
        tix = [0, 0]

        def t32():
            i = tix[0]
            tix[0] = (i + 1) % NT32
            return T32[i], f"t32_{i}"

        cst = sb("cst", [128, 5 * 128 + 4])
        S.dma("sp", cst[:], consts, "cst", w=["cst"])
        ident = cst[:, 0:128]
        UT = cst[:, 128:256]
        ONB = cst[:, 256:384]
        NEGM = cst[:, 384:512]
        SM = cst[:, 512:640]
        pv = cst[:, 640:641]
        kbias = cst[:, 641:642]
        cm0 = cst[:, 642:643]
        cm1 = cst[:, 643:644]
        identb = sb("identb", [128, 128], BF16)
        S.op("dve", lambda e: e.tensor_copy(out=identb[:], in_=ident), r=["cst"], w=["identb"])
        ones32 = sb("ones32", [128, 128])
        S.op("dve", lambda e: e.memset(ones32[:], 1.0), w=["ones32"])
        epsln = sb("epsln", [128, 1])
        S.op("dve", lambda e: e.memset(epsln[:], LN_EPS), w=["epsln"])
        epsrms = sb("epsrms", [128, 1])
        S.op("dve", lambda e: e.memset(epsrms[:], RMS_EPS), w=["epsrms"])
        epsrmsq = sb("epsrmsq", [128, 1])
        S.op("dve", lambda e: e.memset(epsrmsq[:], 128.0 * RMS_EPS), w=["epsrms"])

        small = sb("small", [128, 16 + 8 + 8 + 128 + 36])
        esink = small[:, 0:16]
        negA = small[:, 16:24]
        dtbb = small[:, 24:32]
        normwb = small[:, 32:160]
        brb = small[:, 160:196]
        S.dma("sp", esink, sinks.partition_broadcast(128), "sm", w=["small"])
        S.dma("sp", negA, alog.partition_broadcast(128), "sm", w=["small"])
        S.dma("sp", dtbb, dtb.partition_broadcast(128), "sm", w=["small"])
        S.dma("sp", normwb, normw.partition_broadcast(128), "sm", w=["small"])
        S.dma("sp", brb, br.partition_broadcast(128), "sm", w=["small"])
        S.op("act", lambda e: e.activation(out=esink, in_=esink, func=AF.Exp), r=["small"], w=["small"])
        S.op("act", lambda e: e.activation(out=negA, in_=negA, func=AF.Exp), r=["small"], w=["small"])
        S.op("dve", lambda e: e.tensor_scalar(out=negA, in0=negA, scalar1=-1.0, scalar2=None, op0=ALU.mult),
             r=["small"], w=["small"])
        convw_sb = sb("convw_sb", [128, 24, 4])
        S.dma("sp", convw_sb[:], convw, "cw", w=["convw"])
        wbd_sb = sb("wbd_sb", [128, KC, 16], BF16)
        S.dma("pool", wbd_sb[:], wbd, "wbd", w=["wbd"])
        wr_sb = sb("wr_sb", [128, KC, 36])
        S.dma("sp", wr_sb[:], wr, "wr", w=["wr"])

        NW = 3
        WP = [sb(f"wp{i}", [128, KC, PW], BF16) for i in range(NW)]
        wpi = [0]

        def load_panel(src):
            i = wpi[0]
            wpi[0] = (i + 1) % NW
            S.dma("pool", WP[i][:], src, f"wp{i}", w=[f"wp{i}"])
            return WP[i], f"wp{i}"

        XT = [sb(f"xt{i}", [128, D]) for i in range(2)]
        xti = [0]
        modT = sb("modT", [128, 96])
        es0 = contextlib.ExitStack()

        def sb0(name, shape, dt=F32):
            return es0.enter_context(nc.sbuf_tensor(name, list(shape), dt))

        bT = sb0("bT", [128, 96])
        S.dma("sp", bT[:], badaT, "bT", w=["bT"])
        cTs = sb0("cTs", [128, KC])
        S.dma("sp", cTs[:], cT, "cT", w=["cTs"])
        scb = sb0("scb", [128, KC], BF16)
        S.op("act", lambda e: e.activation(out=scb[:], in_=cTs[:], func=AF.Silu), r=["cTs"], w=["scb"])
        screp = sb0("screp", [128, KC, 128], BF16)
        S.op("dve", lambda e: e.tensor_copy(out=screp[:], in_=scb[:].unsqueeze(2).to_broadcast([128, KC, 128])),
             r=["scb"], w=["screp"])
        gbc = [XT[0], XT[1]]
        S.dma("sp", XT[0][:], badag[0].partition_broadcast(128), "xt0", w=["xt0"])
        S.dma("sp", XT[1][:], badag[1].partition_broadcast(128), "xt1", w=["xt1"])
        for pi in range(48):
            wp, wk = load_panel(wada[pi])
            ps, pk = psn()
            for jj in range(2):
                for k in range(KC):
                    S.op("pe", lambda e, jj=jj, k=k: e.matmul(ps[:, jj:jj + 1], wp[:, k, jj * 128:(jj + 1) * 128],
                                                             scb[:, k:k + 1], start=(k == 0), stop=(k == KC - 1)),
                         r=[wk, "scb"], w=[pk])
            S.op("dve", lambda e: e.tensor_tensor(out=modT[:, pi * 2:pi * 2 + 2], in0=ps[:, 0:2],
                                                  in1=bT[:, pi * 2:pi * 2 + 2], op=ALU.add),
                 r=[pk, "bT"], w=["modT"])
            gi = None
            if 16 <= pi < 24:
                gi, off = 0, (pi - 16) * PW
            elif 40 <= pi < 48:
                gi, off = 1, (pi - 40) * PW
            if gi is not None:
                ps2, pk2 = psn()
                for k in range(KC):
                    S.op("pe", lambda e, k=k: e.matmul(ps2[:, 0:PW], screp[:, k, :], wp[:, k, :],
                                                       start=(k == 0), stop=(k == KC - 1)),
                         r=[wk, "screp"], w=[pk2])
                S.op("dve", lambda e: e.tensor_tensor(out=gbc[gi][:, off:off + PW], in0=ps2[:, 0:PW],
                                                      in1=gbc[gi][:, off:off + PW], op=ALU.add),
                     r=[pk2, f"xt{gi}"], w=[f"xt{gi}"])
        S.op("dve", lambda e: e.tensor_scalar(out=modT[:, 16:32], in0=modT[:, 16:32], scalar1=1.0, scalar2=None,
                                              op0=ALU.add), r=["modT"], w=["modT"])
        S.op("dve", lambda e: e.tensor_scalar(out=modT[:, 64:80], in0=modT[:, 64:80], scalar1=1.0, scalar2=None,
                                              op0=ALU.add), r=["modT"], w=["modT"])
        sh1, sc1, sh2, sc2 = modT[:, 0:16], modT[:, 16:32], modT[:, 48:64], modT[:, 64:80]
        for gi in range(2):
            S.dma("sp", gscr[gi], XT[gi][:], "gscr", r=[f"xt{gi}"], w=["gscr"])
        S.barrier()
        es0.close()

        stt = sb("stt", [128, 4, 6])
        mv = sb("mv", [128, 4])

        def ln_stats(xa, xk):
            for c in range(4):
                S.op("dve", lambda e, c=c: e.bn_stats(out=stt[:, c, :], in_=xa[:, c * 512:(c + 1) * 512]),
                     r=[xk], w=["stt"])
            S.op("dve", lambda e: e.bn_aggr(out=mv[:, 0:2], in_=stt[:].rearrange("p a b -> p (a b)")),
                 r=["stt"], w=["mv"])
            S.op("act", lambda e: e.activation(out=mv[:, 2:3], in_=mv[:, 1:2], func=AF.Sqrt, bias=epsln[:], scale=1.0),
                 r=["mv", "epsln"], w=["mv"])
            S.op("dve", lambda e: e.reciprocal(out=mv[:, 2:3], in_=mv[:, 2:3]), r=["mv"], w=["mv"])

        def ln_normalize(xa, xk, oa, ok):
            S.op("dve", lambda e: e.tensor_scalar(out=oa, in0=xa, scalar1=mv[:, 0:1], scalar2=mv[:, 2:3],
                                                  op0=ALU.subtract, op1=ALU.mult), r=[xk, "mv"], w=[ok])

        def mod_transpose(xa, xk, sc, sh, dst, dk, dst32=None, d32k=None):
            for k4 in range(4):
                ps, pk = psn()
                for q in range(4):
                    k = k4 * 4 + q
                    S.op("pe", lambda e, k=k, q=q: e.transpose(ps[:, q * 128:(q + 1) * 128],
                                                               xa[:, k * 128:(k + 1) * 128], ident),
                         r=[xk, "cst"], w=[pk])
                for q in range(4):
                    k = k4 * 4 + q
                    if dst32 is not None:
                        S.op("dve", lambda e, k=k, q=q: e.tensor_scalar(
                            out=dst32(k), in0=ps[:, q * 128:(q + 1) * 128], scalar1=sc[:, k:k + 1],
                            scalar2=sh[:, k:k + 1], op0=ALU.mult, op1=ALU.add), r=[pk, "modT"], w=[d32k])
                        S.op("act", lambda e, k=k: e.copy(out=dst(k), in_=dst32(k)), r=[d32k], w=[dk])
                    elif q % 2 == 0:
                        S.op("act", lambda e, k=k, q=q: e.activation(
                            out=dst(k), in_=ps[:, q * 128:(q + 1) * 128], func=AF.Identity,
                            bias=sh[:, k:k + 1], scale=sc[:, k:k + 1]), r=[pk, "modT"], w=[dk])
                    else:
                        S.op("dve", lambda e, k=k, q=q: e.tensor_scalar(
                            out=dst(k), in0=ps[:, q * 128:(q + 1) * 128], scalar1=sc[:, k:k + 1],
                            scalar2=sh[:, k:k + 1], op0=ALU.mult, op1=ALU.add), r=[pk, "modT"], w=[dk])

        import os as _os
        _stop = _os.environ.get("K_STOP", "")
        es1 = contextlib.ExitStack()
        sb_outer = sb

        def sb(name, shape, dt=F32):
            return es1.enter_context(nc.sbuf_tensor(name, list(shape), dt))

        g1bc = sb("g1bc", [128, D])
        S.dma("sp", g1bc[:], gscr[0], "g1bc", r=["gscr"], w=["g1bc"])
        uT = sb("uT", [128, KC, 512], BF16)
        QT = sb("QT", [128, 8, 512], BF16)
        KT = sb("KT", [128, 4, 640], BF16)
        VA = sb("VA", [128, 5, 4, 65], BF16)
        S.op("dve", lambda e: e.memset(VA[:], 1.0), w=["VA"])
        S.op("dve", lambda e: e.memset(KT[:], 0.0), w=["KT"])
        hist = sb("hist", [128, 24, 3])
        S.op("dve", lambda e: e.memset(hist[:], 0.0), w=["hist"])
        qkm = sb("qkm", [128, KC, 512], BF16)
        qT = qkm[:, 0:8, :]
        kT = qkm[:, 8:16, :]
        mT = qkm
        vT = sb("vT", [128, 8, 512], BF16)
        zs = sb("zs", [128, 4, 1024], BF16)
        bd = sb("bd", [128, 4, 16])
        S32 = sb("S32", [128, 8, 128])
        Sb = [sb(f"Sb{i}", [128, 8, 128], BF16) for i in range(2)]
        S.op("dve", lambda e: e.memset(S32[:], 0.0), w=["S32"])
        S.op("dve", lambda e: e.memset(Sb[0][:], 0.0), w=["Sb0"])
        sbi = [0]
        PT = [sb(f"PT{i}", [128, 4, 128], BF16) for i in range(4)]
        for i in range(4):
            S.op("dve", lambda e, i=i: e.memset(PT[i][:], 0.0), w=[f"PT{i}"])
        oaun = sb("oaun", [128, 16, 65])
        oa = sb("oa", [128, 1024], BF16)
        ob = sb("ob", [128, 1024], BF16)
        oaT = QT
        obT = vT
        pre = [sb(f"pre{i}", [128, 515]) for i in range(2)]
        prei = [0]
        gsmall = sb("gsm", [128, 4, 96])
        vtok = sb("vtok", [128, 8, 128], BF16)
        kdec = sb("kdec", [128, 8, 128], BF16)
        kdd = sb("kdd", [128, 8, 128], BF16)
        nwT = sb("nwT", [128, 8, 128], BF16)
        up = sb("up", [128, 8, 128])
        vn = sb("vn", [128, 8, 128], BF16)
        oacc = sb("oacc", [128, 8, 128])
        Xb = [sb(f"Xb{i}", [128, 4, 128], BF16) for i in range(4)]
        Yb = [sb(f"Yb{i}", [128, 4, 128], BF16) for i in range(4)]
        Pb = [sb(f"Pb{i}", [128, 4, 128], BF16) for i in range(4)]
        QKm = [sb(f"QKm{i}", [128, 4, 128], BF16) for i in range(2)]

        def dump(name, ap_sb, key, shape):
            if not dbg:
                return
            t = nc.dram_tensor("dbg_" + name, list(shape), F32, kind="ExternalOutput").ap()
            tmp = sb("dbgt_" + name, list(shape))
            S.op("dve", lambda e: e.tensor_copy(out=tmp[:], in_=ap_sb), r=[key], w=["dbgt_" + name])
            S.dma("sp", t, tmp[:], "dbg_" + name, r=["dbgt_" + name])
            dbg_out[name] = True

        if dbg:
            dump("modT", modT[:], "modT", [128, 96])

        def mixer_sb(xsrc, is_prefix, need_kv, first_main, sbi_main):
            for tb in range(4):
                xa = XT[xti[0]]
                xk = f"xt{xti[0]}"
                xti[0] ^= 1
                S.dma("sp", xa[:], xsrc[tb * 128:(tb + 1) * 128, :], xk, w=[xk])
                ln_stats(xa[:], xk)
                ln_normalize(xa[:], xk, xa[:], xk)
                mod_transpose(xa[:], xk, sc1, sh1, lambda k, tb=tb: uT[:, k, tb * 128:(tb + 1) * 128], "uT")

            _st2 = _os.environ.get("K_STOP2", "")
            if _st2 == "s1":
                return

            def fm_tile(panel, col, evac):
                wp, wk = panel
                ps, pk = psn()
                for k in range(KC):
                    S.op("pe", lambda e, k=k: e.matmul(ps[:], wp[:, k, col:col + 128], uT[:, k, :],
                                                       start=(k == 0), stop=(k == KC - 1)), r=[wk, "uT"], w=[pk])
                evac(ps, pk)

            if not is_prefix:
                for p in range(4):
                    pan = load_panel(win[p])
                    for t in range(2):
                        c = p * 2 + t
                        fm_tile(pan, t * 128, lambda ps, pk, c=c: S.op(
                            "act", lambda e: e.copy(out=QT[:, c, :], in_=ps[:]), r=[pk], w=["QT"]))
            if need_kv:
                for p in range(2):
                    pan = load_panel(win[4 + p])
                    for t in range(2):
                        h = p * 2 + t
                        fm_tile(pan, t * 128, lambda ps, pk, h=h: S.op(
                            "dve", lambda e: e.tensor_copy(out=KT[:, h, 128:640], in_=ps[:]), r=[pk], w=["KT"]))
                wp, wk = load_panel(win[6])
                for tb in range(4):
                    ps, pk = psn()
                    for k in range(KC):
                        S.op("pe", lambda e, k=k, tb=tb: e.matmul(ps[:, 0:256], uT[:, k, tb * 128:(tb + 1) * 128],
                                                                  wp[:, k, :], start=(k == 0), stop=(k == KC - 1)),
                             r=[wk, "uT"], w=[pk])
                    S.op("act", lambda e, tb=tb: e.copy(out=VA[:, 1 + tb, :, 0:64],
                                                        in_=ps[:, 0:256].rearrange("p (h d) -> p h d", h=4)),
                         r=[pk], w=["VA"])
            if _st2 == "s2":
                return
            for tb in range(4):
                ps, pk = psn()
                for k in range(KC):
                    S.op("pe", lambda e, k=k, tb=tb: e.matmul(ps[:, 0:16], uT[:, k, tb * 128:(tb + 1) * 128],
                                                              wbd_sb[:, k, :], start=(k == 0), stop=(k == KC - 1)),
                         r=["wbd", "uT"], w=[pk])
                S.op("dve", lambda e, tb=tb: e.tensor_copy(out=bd[:, tb, :], in_=ps[:, 0:16]), r=[pk], w=["bd"])
            for tb in range(4):
                gdn_pre(tb)

            if _st2 == "s3":
                return
            pending = []

            def l2_finish(item):
                sl, slk, sq, sqk, kind, hh = item
                ps2, pk2 = psn()
                S.op("pe", lambda e: e.matmul(ps2[:], ones32[:], sq[:], start=True, stop=True),
                     r=["ones32", sqk], w=[pk2])
                rn, rnk = sq, sqk
                S.op("act", lambda e: e.activation(out=rn[:], in_=ps2[:], func=AF.Sqrt, bias=epsrms[:], scale=1.0),
                     r=[pk2, "epsrms"], w=[rnk])
                S.op("dve", lambda e: e.reciprocal(out=rn[:], in_=rn[:]), r=[rnk], w=[rnk])
                dst, dkk = (qT, "qkm") if kind == 0 else (kT, "qkm")
                scl = 128.0 ** -0.5 if kind == 0 else 1.0
                S.op("dve", lambda e: e.scalar_tensor_tensor(out=dst[:, hh, :], in0=sl[:], scalar=scl, in1=rn[:],
                                                             op0=ALU.mult, op1=ALU.mult), r=[slk, rnk], w=[dkk])

            for ft in range(24):
                if is_prefix and ft < 8:
                    continue
                if ft % 2 == 0:
                    pan = load_panel(win[7 + ft // 2])
                kind = ft // 8
                hh = ft % 8

                def evac(ps, pk, ft=ft, kind=kind, hh=hh):
                    pr = pre[prei[0]]
                    prk = f"pre{prei[0]}"
                    prei[0] ^= 1
                    S.op("dve", lambda e: e.tensor_copy(out=pr[:, 0:3], in_=hist[:, ft, :]), r=["hist"], w=[prk])
                    if is_prefix:
                        S.op("act", lambda e: e.activation(out=pr[:, 3:515], in_=ps[:], func=AF.Copy, scale=pv),
                             r=[pk, "cst"], w=[prk])
                    else:
                        S.op("act", lambda e: e.copy(out=pr[:, 3:515], in_=ps[:]), r=[pk], w=[prk])
                    S.op("dve", lambda e: e.tensor_copy(out=hist[:, ft, :], in_=pr[:, 512:515]), r=[prk], w=["hist"])
                    y, yk = t32()
                    S.op("dve", lambda e: e.tensor_scalar(out=y[:], in0=pr[:, 0:512], scalar1=convw_sb[:, ft, 0:1],
                                                          scalar2=None, op0=ALU.mult), r=[prk, "convw"], w=[yk])
                    for i in range(1, 4):
                        S.op("dve", lambda e, i=i: e.scalar_tensor_tensor(
                            out=y[:], in0=pr[:, i:i + 512], scalar=convw_sb[:, ft, i:i + 1], in1=y[:],
                            op0=ALU.mult, op1=ALU.add), r=[prk, "convw", yk], w=[yk])
                    if kind == 2:
                        S.op("act", lambda e: e.activation(out=vT[:, hh, :], in_=y[:], func=AF.Silu), r=[yk], w=["vT"])
                        return
                    sl, slk = y, yk
                    S.op("act", lambda e: e.activation(out=sl[:], in_=y[:], func=AF.Silu), r=[yk], w=[slk])
                    sq, sqk = t32()
                    S.op("dve", lambda e: e.tensor_tensor(out=sq[:], in0=sl[:], in1=sl[:], op=ALU.mult),
                         r=[slk], w=[sqk])
                    pending.append((sl, slk, sq, sqk, kind, hh))

                fm_tile(pan, (ft % 2) * 128, evac)
                while len(pending) > (0 if kind == 2 else 1):
                    l2_finish(pending.pop(0))
            while pending:
                l2_finish(pending.pop(0))

            if _st2 == "s4":
                return
            if not is_prefix:
                for p in range(4):
                    wp, wk = load_panel(win[19 + p])
                    for tb in range(4):
                        ps, pk = psn()
                        for k in range(KC):
                            S.op("pe", lambda e, k=k, tb=tb: e.matmul(ps[:, 0:256], uT[:, k, tb * 128:(tb + 1) * 128],
                                                                      wp[:, k, :], start=(k == 0), stop=(k == KC - 1)),
                                 r=[wk, "uT"], w=[pk])
                        S.op("act", lambda e, tb=tb, p=p: e.activation(out=zs[:, tb, p * 256:(p + 1) * 256],
                                                                       in_=ps[:, 0:256], func=AF.Silu),
                             r=[pk], w=["zs"])

            for tb in range(4):
                tc0 = tb * 128
                if not is_prefix:
                    attention_tb(tb, first_main and tb == 0)
                gdn_tb(tb, is_prefix)
            if need_kv:
                S.op("pool", lambda e: e.tensor_copy(out=KT[:, :, 0:128], in_=KT[:, :, 512:640]), r=["KT"], w=["KT"])
                S.op("pool", lambda e: e.tensor_copy(out=VA[:, 0, :, :], in_=VA[:, 4, :, :]), r=["VA"], w=["VA"])
            if is_prefix:
                return
            for i in range(KC):
                gp, gk = load_panel(win[23 + i])
                if i % 2 == 0:
                    pp, ppk = load_panel(wproj[i // 2])
                col = (i % 2) * 128
                psA, pkA = psn()
                for k in range(KC):
                    S.op("pe", lambda e, k=k: e.matmul(psA[:], gp[:, k, 0:128], uT[:, k, :], start=(k == 0),
                                                       stop=(k == KC - 1)), r=[gk, "uT"], w=[pkA])
                sgA, sgAk = t32()
                S.op("act", lambda e: e.activation(out=sgA[:], in_=psA[:], func=AF.Sigmoid), r=[pkA], w=[sgAk])
                psB, pkB = psn()
                for k in range(KC):
                    S.op("pe", lambda e, k=k: e.matmul(psB[:], gp[:, k, 128:256], uT[:, k, :], start=(k == 0),
                                                       stop=(k == KC - 1)), r=[gk, "uT"], w=[pkB])
                sgB, sgBk = t32()
                S.op("act", lambda e: e.activation(out=sgB[:], in_=psB[:], func=AF.Sigmoid), r=[pkB], w=[sgBk])
                psC, pkC = psn()
                for k in range(8):
                    S.op("pe", lambda e, k=k: e.matmul(psC[:], pp[:, k, col:col + 128], oaT[:, k, :], start=(k == 0),
                                                       stop=(k == 7)), r=[ppk, "QT"], w=[pkC])
                S.op("dve", lambda e: e.tensor_tensor(out=sgA[:], in0=sgA[:], in1=psC[:], op=ALU.mult),
                     r=[sgAk, pkC], w=[sgAk])
                psD, pkD = psn()
                for k in range(8):
                    S.op("pe", lambda e, k=k: e.matmul(psD[:], pp[:, 8 + k, col:col + 128], obT[:, k, :],
                                                       start=(k == 0), stop=(k == 7)), r=[ppk, "vT"], w=[pkD])
                S.op("dve", lambda e: e.tensor_tensor(out=sgB[:], in0=sgB[:], in1=psD[:], op=ALU.mult),
                     r=[sgBk, pkD], w=[sgBk])
                S.op("dve", lambda e, i=i: e.tensor_tensor(out=mT[:, i, :], in0=sgA[:], in1=sgB[:], op=ALU.add),
                     r=[sgAk, sgBk], w=["qkm"])
            for half in range(2):
                for j in range(2):
                    tb = half * 2 + j
                    tok0 = sbi_main * 512 + tb * 128
                    S.dma("sp", XT[j][:], xm[tok0:tok0 + 128, :], f"xt{j}", w=[f"xt{j}"])
                for n in range(8):
                    wo, wok = load_panel(wout[n])
                    for j in range(2):
                        tb = half * 2 + j
                        ps, pk = psn()
                        for k in range(KC):
                            S.op("pe", lambda e, k=k, tb=tb: e.matmul(ps[:, 0:PW], mT[:, k, tb * 128:(tb + 1) * 128],
                                                                      wo[:, k, :], start=(k == 0), stop=(k == KC - 1)),
                                 r=[wok, "qkm"], w=[pk])
                        tt, ttk = t32()
                        S.op("dve", lambda e, n=n: e.tensor_tensor(out=tt[:, 0:PW], in0=ps[:, 0:PW],
                                                                   in1=g1bc[:, n * PW:(n + 1) * PW], op=ALU.mult),
                             r=[pk, "g1bc"], w=[ttk])
                        S.op("dve", lambda e, n=n, j=j: e.scalar_tensor_tensor(
                            out=XT[j][:, n * PW:(n + 1) * PW], in0=XT[j][:, n * PW:(n + 1) * PW], scalar=ALPHA,
                            in1=tt[:, 0:PW], op0=ALU.mult, op1=ALU.add), r=[ttk, f"xt{j}"], w=[f"xt{j}"])
                for j in range(2):
                    tb = half * 2 + j
                    tok0 = sbi_main * 512 + tb * 128
                    S.dma("sp", x1s[tok0:tok0 + 128, :], XT[j][:], f"x1st{j}", r=[f"xt{j}"], w=["x1s"])

        def attention_tb(tb, masked_halo):
            tc0 = tb * 128
            for kvh in range(4):
                pt0, pt0k = PT[(kvh % 2) * 2], f"PT{(kvh % 2) * 2}"
                pt1, pt1k = PT[(kvh % 2) * 2 + 1], f"PT{(kvh % 2) * 2 + 1}"
                psA, pkA_ = psn()
                psB, pkB_ = psn()
                for kb, kc0 in ((0, tc0), (1, tc0 + 128)):
                    for half, (ps, pk) in enumerate(((psA, pkA_), (psB, pkB_))):
                        pr = slice(half * 64, half * 64 + 64)
                        S.op("pe", lambda e, ps=ps, pr=pr, kc0=kc0, kb=kb: e.matmul(
                            ps[:, kb * 256:(kb + 1) * 256].rearrange("p (a b) -> p a b", a=2),
                            KT[pr, kvh, kc0:kc0 + 128], QT[pr, 2 * kvh:2 * kvh + 2, tc0:tc0 + 128],
                            start=True, stop=True), r=["KT", "QT"], w=[pk])
                for (ps, pk, sl) in ((psA, pkA_, slice(0, 2)), (psB, pkB_, slice(2, 4))):
                    v3 = ps[:].rearrange("p (kb s q) -> p kb s q", kb=2, s=2)
                    S.op("act", lambda e, v3=v3, sl=sl: e.activation(
                        out=pt0[0:64, sl, 0:64], in_=v3[0:64, 0, :, 0:64], func=AF.Exp,
                        bias=(kbias[0:64] if masked_halo else 0.0), scale=0.125), r=[pk, "cst"], w=[pt0k])
                    S.op("act", lambda e, v3=v3, sl=sl: e.activation(
                        out=pt0[64:128, sl, :], in_=v3[64:128, 0, :, :], func=AF.Exp,
                        bias=(kbias[64:128] if masked_halo else 0.0), scale=0.125), r=[pk, "cst"], w=[pt0k])
                    S.op("act", lambda e, v3=v3, sl=sl: e.activation(
                        out=pt1[0:64, sl, :], in_=v3[0:64, 1, :, :], func=AF.Exp, scale=0.125),
                        r=[pk], w=[pt1k])
                    S.op("act", lambda e, v3=v3, sl=sl: e.activation(
                        out=pt1[64:128, sl, 64:128], in_=v3[64:128, 1, :, 64:128], func=AF.Exp, scale=0.125),
                        r=[pk], w=[pt1k])
                pso, pko = psn()
                for s in range(4):
                    S.op("pe", lambda e, s=s: e.matmul(pso[:, s * 65:(s + 1) * 65], pt0[:, s, :], VA[:, tb, kvh, :],
                                                       start=True, stop=False), r=[pt0k, "VA"], w=[pko])
                    S.op("pe", lambda e, s=s: e.matmul(pso[:, s * 65:(s + 1) * 65], pt1[:, s, :], VA[:, tb + 1, kvh, :],
                                                       start=False, stop=True), r=[pt1k, "VA"], w=[pko])
                S.op("dve", lambda e: e.tensor_copy(out=oaun[:, kvh * 4:(kvh + 1) * 4, :],
                                                    in_=pso[:, 0:260].rearrange("p (s d) -> p s d", s=4)),
                     r=[pko], w=["oaun"])
            gsm = gsmall[:, tb, :]
            den = gsm[:, 64:80]
            S.op("dve", lambda e: e.tensor_tensor(out=den, in0=oaun[:, :, 64], in1=esink, op=ALU.add),
                 r=["oaun", "small"], w=[f"gsm_den{tb}"])
            S.op("dve", lambda e: e.reciprocal(out=den, in_=den), r=[f"gsm_den{tb}"], w=[f"gsm_den{tb}"])
            S.op("dve", lambda e: e.tensor_tensor(out=oa[:].rearrange("p (h d) -> p h d", h=16), in0=oaun[:, :, 0:64],
                                                  in1=den.unsqueeze(2).to_broadcast([128, 16, 64]), op=ALU.mult),
                 r=["oaun", f"gsm_den{tb}"], w=["oa"])
            psb, pkb = psn()
            psv = psb[:].bitcast(BF16)
            for c in range(8):
                S.op("pe", lambda e, c=c: e.transpose(psv[:, c * 128:(c + 1) * 128], oa[:, c * 128:(c + 1) * 128],
                                                      identb[:]), r=["oa", "identb"], w=[pkb])
            S.op("act", lambda e: e.copy(out=oaT[:, :, tc0:tc0 + 128],
                                         in_=psv[:, 0:1024].rearrange("p (c t) -> p c t", c=8)), r=[pkb], w=["QT"])

        def gdn_pre(tb):
            gsm = gsmall[:, tb, :]
            beta = gsm[:, 0:8]
            g = gsm[:, 8:16]
            cum = gsm[:, 16:24]
            ecum = gsm[:, 24:32]
            ekd = gsm[:, 32:40]
            gm = gsm[:, 40:56]
            decbc = gsm[:, 80:96]
            S.op("act", lambda e: e.activation(out=beta, in_=bd[:, tb, 0:8], func=AF.Sigmoid), r=["bd"], w=["g_beta"])
            tmp = gsm[:, 56:64]
            S.op("dve", lambda e: e.tensor_tensor(out=g, in0=bd[:, tb, 8:16], in1=dtbb, op=ALU.add),
                 r=["bd", "small"], w=["g_g"])
            S.op("dve", lambda e: e.scalar_tensor_tensor(out=tmp, in0=g, scalar=-1.0, in1=g, op0=ALU.mult,
                                                         op1=ALU.max), r=["g_g"], w=["g_tmp"])
            S.op("act", lambda e: e.activation(out=tmp, in_=tmp, func=AF.Exp, scale=-1.0), r=["g_tmp"], w=["g_tmp"])
            S.op("dve", lambda e: e.tensor_scalar(out=tmp, in0=tmp, scalar1=1.0, scalar2=None, op0=ALU.add),
                 r=["g_tmp"], w=["g_tmp"])
            S.op("act", lambda e: e.activation(out=tmp, in_=tmp, func=AF.Ln), r=["g_tmp"], w=["g_tmp"])
            S.op("dve", lambda e: e.scalar_tensor_tensor(out=g, in0=g, scalar=0.0, in1=tmp, op0=ALU.max, op1=ALU.add),
                 r=["g_g", "g_tmp"], w=["g_g"])
            S.op("dve", lambda e: e.tensor_tensor(out=g, in0=g, in1=negA, op=ALU.mult), r=["g_g", "small"], w=["g_g"])
            S.op("dve", lambda e: e.tensor_scalar(out=gm[:, 0:8], in0=g, scalar1=cm0, scalar2=None, op0=ALU.mult),
                 r=["g_g", "cst"], w=["g_gm"])
            S.op("dve", lambda e: e.tensor_scalar(out=gm[:, 8:16], in0=g, scalar1=cm1, scalar2=None, op0=ALU.mult),
                 r=["g_g", "cst"], w=["g_gm"])
            psc, pkc = psn()
            S.op("pe", lambda e: e.matmul(psc[:, 0:8], UT, g, start=True, stop=True), r=["cst", "g_g"], w=[pkc])
            S.op("pe", lambda e: e.matmul(psc[:, 8:16], ONB, g, start=True, stop=True), r=["cst", "g_g"], w=[pkc])
            S.op("pe", lambda e: e.matmul(psc[:, 16:32], ones32[:], gm, start=True, stop=True),
                 r=["ones32", "g_gm"], w=[pkc])
            S.op("dve", lambda e: e.tensor_copy(out=cum, in_=psc[:, 0:8]), r=[pkc], w=["g_cum"])
            S.op("act", lambda e: e.activation(out=ecum, in_=psc[:, 0:8], func=AF.Exp), r=[pkc], w=["g_ecum"])
            S.op("dve", lambda e: e.tensor_tensor(out=ekd, in0=psc[:, 8:16], in1=cum, op=ALU.subtract),
                 r=[pkc, "g_cum"], w=["g_ekd"])
            S.op("act", lambda e: e.activation(out=ekd, in_=ekd, func=AF.Exp), r=["g_ekd"], w=["g_ekd"])
            S.op("act", lambda e: e.activation(out=decbc, in_=psc[:, 16:32], func=AF.Exp), r=[pkc], w=["g_dec"])

        def gdn_tb(tb, is_prefix):
            tc0 = tb * 128
            gsm = gsmall[:, tb, :]
            beta = gsm[:, 0:8]
            g = gsm[:, 8:16]
            cum = gsm[:, 16:24]
            ecum = gsm[:, 24:32]
            ekd = gsm[:, 32:40]
            decbc = gsm[:, 80:96]
            _st3 = ""
            for grp in range(2):
                psb, pkb = psn()
                psv = psb[:].bitcast(BF16)
                for hq in range(4):
                    h = grp * 4 + hq
                    S.op("pe", lambda e, h=h, hq=hq: e.transpose(psv[:, hq * 128:(hq + 1) * 128],
                                                                 kT[:, h, tc0:tc0 + 128], identb[:]),
                         r=["qkm", "identb"], w=[pkb])
                    S.op("pe", lambda e, h=h, hq=hq: e.transpose(psv[:, 512 + hq * 128:512 + (hq + 1) * 128],
                                                                 vT[:, h, tc0:tc0 + 128], identb[:]),
                         r=["vT", "identb"], w=[pkb])
                hs = slice(grp * 4, grp * 4 + 4)
                _sk = _os.environ.get("K_SKIP", "")
                if _sk == "noevac":
                    continue
                kv_ = psv[:, 0:512].rearrange("p (h d) -> p h d", h=4)
                if _sk in ("", "only_kdec"):
                  S.op("dve", lambda e: e.tensor_tensor(out=kdec[:, hs, :], in0=kv_,
                                                      in1=ecum[:, hs].unsqueeze(2).to_broadcast([128, 4, 128]),
                                                      op=ALU.mult), r=[pkb, "g_ecum"], w=["kdec"])
                ekb = gsm[:, 56:64]
                S.op("dve", lambda e: e.tensor_tensor(out=ekb, in0=ekd, in1=beta, op=ALU.mult),
                     r=["g_ekd", "g_beta", "g_tmp"], w=["g_tmp"])
                if _sk in ("", "only_kdd"):
                  S.op("dve", lambda e: e.tensor_tensor(out=kdd[:, hs, :], in0=kv_,
                                                      in1=ekb[:, hs].unsqueeze(2).to_broadcast([128, 4, 128]),
                                                      op=ALU.mult), r=[pkb, "g_tmp"], w=["kdd"])
                if _sk in ("", "only_vtok"):
                  S.op("act", lambda e: e.copy(out=vtok[:, hs, :],
                                             in_=psv[:, 512:1024].rearrange("p (h d) -> p h d", h=4)),
                     r=[pkb], w=["vtok"])
            if _st3 == "g2":
                return
            Pfin = []
            for grp in range(2):
                hs = slice(grp * 4, grp * 4 + 4)
                GU, GUk = t32()
                S.op("dve", lambda e: e.tensor_tensor(
                    out=GU[:].rearrange("p (h c) -> p h c", h=4), in0=UT.unsqueeze(1).to_broadcast([128, 4, 128]),
                    in1=g[:, hs].unsqueeze(2).to_broadcast([128, 4, 128]), op=ALU.mult),
                    r=["cst", "g_g"], w=[GUk])
                psr, pkr = psn()
                S.op("pe", lambda e: e.matmul(psr[:], ONB, GU[:], start=True, stop=True), r=["cst", GUk], w=[pkr])
                E, Ek = t32()
                for hq in range(4):
                    h = grp * 4 + hq
                    S.op("dve", lambda e, h=h, hq=hq: e.scalar_tensor_tensor(
                        out=E[:, hq * 128:(hq + 1) * 128], in0=psr[:, hq * 128:(hq + 1) * 128],
                        scalar=cum[:, h:h + 1], in1=NEGM, op0=ALU.subtract, op1=ALU.min),
                        r=[pkr, "g_cum", "cst"], w=[Ek])
                S.op("act", lambda e: e.activation(out=E[:], in_=E[:], func=AF.Exp), r=[Ek], w=[Ek])
                E3 = E[:].rearrange("p (h c) -> p h c", h=4)
                S.op("dve", lambda e: e.tensor_tensor(out=E3, in0=E3,
                                                      in1=beta[:, hs].unsqueeze(2).to_broadcast([128, 4, 128]),
                                                      op=ALU.mult), r=[Ek, "g_beta"], w=[Ek])
                if not is_prefix:
                    psq, pkq = psn()
                    for hq in range(4):
                        h = grp * 4 + hq
                        S.op("pe", lambda e, h=h, hq=hq: e.matmul(psq[:, hq * 128:(hq + 1) * 128],
                                                                  kT[:, h, tc0:tc0 + 128], qT[:, h, tc0:tc0 + 128],
                                                                  start=True, stop=True), r=["qkm", "qkm"], w=[pkq])
                    S.op("dve", lambda e: e.tensor_tensor(out=QKm[grp][:].rearrange("p h c -> p (h c)"), in0=psq[:],
                                                          in1=E[:], op=ALU.mult), r=[pkq, Ek], w=[f"QKm{grp}"])
                psk, pkk = psn()
                for hq in range(4):
                    h = grp * 4 + hq
                    S.op("pe", lambda e, h=h, hq=hq: e.matmul(psk[:, hq * 128:(hq + 1) * 128],
                                                              kT[:, h, tc0:tc0 + 128], kT[:, h, tc0:tc0 + 128],
                                                              start=True, stop=True), r=["qkm"], w=[pkk])
                S.op("pool", lambda e: e.tensor_tensor(out=E3, in0=E3, in1=SM.unsqueeze(1).to_broadcast([128, 4, 128]),
                                                       op=ALU.mult), r=[Ek, "cst"], w=[Ek])
                xi = grp * 2
                X, Xk = Xb[xi], f"Xb{xi}"
                S.op("dve", lambda e: e.tensor_tensor(out=X[:].rearrange("p h c -> p (h c)"), in0=psk[:], in1=E[:],
                                                      op=ALU.mult), r=[pkk, Ek], w=[Xk])
                pst, pkt = psn()
                ptv = pst[:].bitcast(BF16)
                for hq in range(4):
                    S.op("pe", lambda e, hq=hq: e.transpose(ptv[:, hq * 128:(hq + 1) * 128], X[:, hq, :], identb[:]),
                         r=[Xk, "identb"], w=[pkt])
                Y, Yk = Yb[xi], f"Yb{xi}"
                S.op("act", lambda e: e.copy(out=Y[:].rearrange("p h c -> p (h c)"), in_=ptv[:, 0:512]),
                     r=[pkt], w=[Yk])
                P_, Pk = Pb[xi], f"Pb{xi}"
                S.op("pool", lambda e: e.tensor_tensor(out=P_[:], in0=identb[:].unsqueeze(1).to_broadcast([128, 4, 128]),
                                                       in1=X[:], op=ALU.subtract), r=["identb", Xk], w=[Pk])
            for m in range(1, 6):
                for grp in range(2):
                    xi = grp * 2
                    nx = xi + (m % 2)
                    cx = xi + ((m + 1) % 2)
                    Xc, Xck, Yc, Yck = Xb[cx], f"Xb{cx}", Yb[cx], f"Yb{cx}"
                    Xn, Xnk, Yn, Ynk = Xb[nx], f"Xb{nx}", Yb[nx], f"Yb{nx}"
                    psy, pky = psn()
                    for hq in range(4):
                        S.op("pe", lambda e, hq=hq: e.matmul(psy[:, hq * 128:(hq + 1) * 128], Xc[:, hq, :],
                                                             Yc[:, hq, :], start=True, stop=True),
                             r=[Xck, Yck], w=[pky])
                    if m < 5:
                        psx, pkx = psn()
                        for hq in range(4):
                            S.op("pe", lambda e, hq=hq: e.matmul(psx[:, hq * 128:(hq + 1) * 128], Yc[:, hq, :],
                                                                 Xc[:, hq, :], start=True, stop=True),
                                 r=[Xck, Yck], w=[pkx])
                    S.op("act", lambda e: e.copy(out=Yn[:].rearrange("p h c -> p (h c)"), in_=psy[:]),
                         r=[pky], w=[Ynk])
                    if m < 5:
                        S.op("dve", lambda e: e.tensor_copy(out=Xn[:].rearrange("p h c -> p (h c)"), in_=psx[:]),
                             r=[pkx], w=[Xnk])
                for grp in range(2):
                    xi = grp * 2
                    nx = xi + (m % 2)
                    cx = xi + ((m + 1) % 2)
                    Yn, Ynk = Yb[nx], f"Yb{nx}"
                    Pc, Pck = Pb[cx], f"Pb{cx}"
                    Pn, Pnk = Pb[nx], f"Pb{nx}"
                    psp, pkp = psn()
                    for hq in range(4):
                        S.op("pe", lambda e, hq=hq: e.matmul(psp[:, hq * 128:(hq + 1) * 128], Yn[:, hq, :],
                                                             Pc[:, hq, :], start=True, stop=True),
                             r=[Ynk, Pck], w=[pkp])
                    S.op("dve", lambda e: e.tensor_tensor(out=Pn[:].rearrange("p h c -> p (h c)"), in0=psp[:],
                                                          in1=Pc[:].rearrange("p h c -> p (h c)"), op=ALU.add),
                         r=[pkp, Pck], w=[Pnk])
            for grp in range(2):
                Pfin.append((Pb[grp * 2 + 1], f"Pb{grp * 2 + 1}"))
            if _st3 == "g3":
                return
            for grp in range(2):
                P_, Pk = Pfin[grp]
                psu, pku = psn()
                psw, pkw = psn()
                for hq in range(4):
                    h = grp * 4 + hq
                    S.op("pe", lambda e, h=h, hq=hq: e.matmul(psu[:, hq * 128:(hq + 1) * 128], P_[:, hq, :],
                                                              vtok[:, h, :], start=True, stop=True),
                         r=[Pk, "vtok"], w=[pku])
                    S.op("pe", lambda e, h=h, hq=hq: e.matmul(psw[:, hq * 128:(hq + 1) * 128], kdec[:, h, :],
                                                              P_[:, hq, :], start=True, stop=True),
                         r=[Pk, "kdec"], w=[pkw])
                hs = slice(grp * 4, grp * 4 + 4)
                S.op("act", lambda e: e.copy(out=up[:, hs, :].rearrange("p h c -> p (h c)"), in_=psu[:]),
                     r=[pku], w=["up"])
                S.op("act", lambda e: e.mul(out=nwT[:, hs, :].rearrange("p h c -> p (h c)"), in_=psw[:], mul=-1.0),
                     r=[pkw], w=["nwT"])
            if _st3 == "g4":
                return
            for ci in range(2):
                pr = slice(ci * 64, ci * 64 + 64)
                cur = sbi[0]
                Sc, Sck = Sb[cur], f"Sb{cur}"
                Sn, Snk = Sb[cur ^ 1], f"Sb{cur ^ 1}"
                pws = []
                for grp in range(2):
                    psw, pkw = psn()
                    pws.append((psw, pkw))
                    for hq in range(4):
                        h = grp * 4 + hq
                        S.op("pe", lambda e, h=h, hq=hq: e.matmul(psw[:, hq * 128:(hq + 1) * 128], nwT[:, h, :],
                                                                  Sc[:, h, :], start=True, stop=True),
                             r=["nwT", Sck], w=[pkw])
                for grp in range(2):
                    psw, pkw = pws[grp]
                    hs = slice(grp * 4, grp * 4 + 4)
                    S.op("dve", lambda e: e.tensor_tensor(out=vn[pr, hs, :].rearrange("p h c -> p (h c)"),
                                                          in0=psw[pr, :],
                                                          in1=up[pr, hs, :].rearrange("p h c -> p (h c)"), op=ALU.add),
                         r=[pkw, "up"], w=["vn"])
                if not is_prefix:
                    for grp in range(2):
                        pso, pko = psn()
                        hs = slice(grp * 4, grp * 4 + 4)
                        for hq in range(4):
                            h = grp * 4 + hq
                            S.op("pe", lambda e, h=h, hq=hq: e.matmul(pso[:, hq * 128:(hq + 1) * 128],
                                                                      qT[:, h, tc0:tc0 + 128], Sc[:, h, :],
                                                                      start=True, stop=True), r=["qkm", Sck], w=[pko])
                        S.op("dve", lambda e: e.tensor_tensor(
                            out=oacc[pr, hs, :], in0=pso[pr, :].rearrange("p (h c) -> p h c", h=4),
                            in1=ecum[pr, hs].unsqueeze(2).to_broadcast([64, 4, 128]), op=ALU.mult),
                            r=[pko, "g_ecum"], w=["oacc"])
                pus = []
                for grp in range(2):
                    psu, pku = psn()
                    pus.append((psu, pku))
                    for hq in range(4):
                        h = grp * 4 + hq
                        S.op("pe", lambda e, h=h, hq=hq: e.matmul(psu[:, hq * 128:(hq + 1) * 128], kdd[pr, h, :],
                                                                  vn[pr, h, :], start=True, stop=True),
                             r=["kdd", "vn"], w=[pku])
                for grp in range(2):
                    psu, pku = pus[grp]
                    hs = slice(grp * 4, grp * 4 + 4)
                    S.op("dve", lambda e: e.tensor_tensor(
                        out=S32[:, hs, :], in0=S32[:, hs, :],
                        in1=decbc[:, ci * 8 + grp * 4:ci * 8 + grp * 4 + 4].unsqueeze(2).to_broadcast([128, 4, 128]),
                        op=ALU.mult), r=["S32", "g_dec"], w=["S32"])
                    S.op("dve", lambda e: e.tensor_tensor(out=S32[:, hs, :].rearrange("p h c -> p (h c)"),
                                                          in0=S32[:, hs, :].rearrange("p h c -> p (h c)"),
                                                          in1=psu[:], op=ALU.add), r=["S32", pku], w=["S32"])
                    S.op("act", lambda e: e.copy(out=Sn[:, hs, :], in_=S32[:, hs, :]), r=["S32"], w=[Snk])
                sbi[0] ^= 1
            if is_prefix:
                return
            for grp in range(2):
                hs = slice(grp * 4, grp * 4 + 4)
                pso, pko = psn()
                for hq in range(4):
                    h = grp * 4 + hq
                    S.op("pe", lambda e, h=h, hq=hq: e.matmul(pso[:, hq * 128:(hq + 1) * 128], QKm[grp][:, hq, :],
                                                              vn[:, h, :], start=True, stop=True),
                         r=[f"QKm{grp}", "vn"], w=[pko])
                S.op("dve", lambda e: e.tensor_tensor(out=oacc[:, hs, :].rearrange("p h c -> p (h c)"),
                                                      in0=oacc[:, hs, :].rearrange("p h c -> p (h c)"), in1=pso[:],
                                                      op=ALU.add), r=["oacc", pko], w=["oacc"])
            sq, sqk = t32()
            sq2, sq2k = t32()
            ms = gsm[:, 56:64]
            for grp, (sqq, sqqk) in enumerate(((sq, sqk), (sq2, sq2k))):
                hs = slice(grp * 4, grp * 4 + 4)
                S.op("pool", lambda e, sqq=sqq: e.tensor_tensor(out=sqq[:].rearrange("p (h c) -> p h c", h=4),
                                                                in0=oacc[:, hs, :], in1=oacc[:, hs, :], op=ALU.mult),
                     r=["oacc"], w=[sqqk])
                S.op("dve", lambda e, sqq=sqq: e.reduce_sum(out=ms[:, hs], in_=sqq[:].rearrange("p (h c) -> p h c", h=4),
                                                            axis=AX.X), r=[sqqk, "g_tmp"], w=["g_tmp"])
            S.op("act", lambda e: e.activation(out=ms, in_=ms, func=AF.Sqrt, bias=epsrms[:], scale=1.0 / 128.0),
                 r=["g_tmp", "epsrms"], w=["g_tmp"])
            S.op("dve", lambda e: e.reciprocal(out=ms, in_=ms), r=["g_tmp"], w=["g_tmp"])
            S.op("pool", lambda e: e.tensor_tensor(out=zs[:, tb, :].rearrange("p (h c) -> p h c", h=8),
                                                   in0=zs[:, tb, :].rearrange("p (h c) -> p h c", h=8),
                                                   in1=normwb.unsqueeze(1).to_broadcast([128, 8, 128]), op=ALU.mult),
                 r=["zs", "small"], w=["zs"])
            S.op("dve", lambda e: e.tensor_tensor(out=oacc[:], in0=oacc[:],
                                                  in1=ms.unsqueeze(2).to_broadcast([128, 8, 128]), op=ALU.mult),
                 r=["oacc", "g_tmp"], w=["oacc"])
            S.op("dve", lambda e: e.tensor_tensor(out=ob[:].rearrange("p (h c) -> p h c", h=8), in0=oacc[:],
                                                  in1=zs[:, tb, :].rearrange("p (h c) -> p h c", h=8), op=ALU.mult),
                 r=["oacc", "zs"], w=["ob"])
            psb, pkb = psn()
            psv = psb[:].bitcast(BF16)
            for c in range(8):
                S.op("pe", lambda e, c=c: e.transpose(psv[:, c * 128:(c + 1) * 128], ob[:, c * 128:(c + 1) * 128],
                                                      identb[:]), r=["ob", "identb"], w=[pkb])
            S.op("act", lambda e: e.copy(out=obT[:, :, tc0:tc0 + 128],
                                         in_=psv[:, 0:1024].rearrange("p (c t) -> p c t", c=8)), r=[pkb], w=["vT"])

        for s in range(NSBP):
            if _stop in ("p0",):
                break
            mixer_sb(xp[s * 512:(s + 1) * 512, :], True, s == NSBP - 1, False, -1)
        for s in range(NSBM):
            if _stop in ("p0", "pp"):
                break
            mixer_sb(xm[s * 512:(s + 1) * 512, :], False, True, s == 0, s)

        S.barrier()
        es1.close()
        es15 = contextlib.ExitStack()

        def sb(name, shape, dt=F32):
            return es15.enter_context(nc.sbuf_tensor(name, list(shape), dt))

        lnb1 = sb("lnb1", [128, 2, D])
        for i in range(2):
            S.dma("sp", lnb1[:, i, :], lnv[i].partition_broadcast(128), "lnb1", w=["lnb1"])
        for tbi in range(NTB):
            if _stop in ("p0", "pp", "p1"):
                break
            xa = XT[xti[0]]
            xk = f"xt{xti[0]}"
            xti[0] ^= 1
            S.dma("sp", xa[:], x1s[tbi * 128:(tbi + 1) * 128, :], xk, r=["x1s"], w=[xk])
            ln_stats(xa[:], xk)
            ln_normalize(xa[:], xk, xa[:], xk)
            S.op("dve", lambda e: e.tensor_tensor(out=xa[:], in0=xa[:], in1=lnb1[:, 0, :], op=ALU.mult),
                 r=[xk, "lnb1"], w=[xk])
            S.op("pool", lambda e: e.tensor_tensor(out=xa[:], in0=xa[:], in1=lnb1[:, 1, :], op=ALU.add),
                 r=[xk, "lnb1"], w=[xk])
            S.dma("sp", x1s[tbi * 128:(tbi + 1) * 128, :], xa[:], f"x1st{xk}", r=[xk], w=["x1s"])
        S.barrier()
        es15.close()
        es2 = contextlib.ExitStack()

        def sb(name, shape, dt=F32):
            return es2.enter_context(nc.sbuf_tensor(name, list(shape), dt))

        GT = TOKM // NG
        GTB = GT // 128
        g2bc = sb("g2bc", [128, D])
        S.dma("sp", g2bc[:], gscr[1], "g2bc", r=["gscr"], w=["g2bc"])
        u2T = sb("u2T", [128, KC, GT], BF16)
        u32 = sb("u32", [128, KC, 128])
        yacc = sb("yacc", [128, GTB, D])
        comb = sb("comb", [128, GTB, NEXP])
        lnb = sb("lnb", [128, 2, D])
        for i in range(2):
            S.dma("sp", lnb[:, i, :], lnv[2 + i].partition_broadcast(128), "lnb", w=["lnb"])
        actT = sb("actT", [128, 4, GT], BF16)
        rt = sb("rt", [128, 128])

        def load_x1(tok0, xa, xk):
            S.dma("sp", xa[:], x1s[tok0:tok0 + 128, :], xk, r=["x1s"], w=[xk])

        for gI in range(NG):
            if _stop in ("p0", "pp", "p1", "p15"):
                break
            for tb in range(GTB):
                tok0 = gI * GT + tb * 128
                xa = XT[xti[0]]
                xk = f"xt{xti[0]}"
                xti[0] ^= 1
                load_x1(tok0, xa, xk)
                ln_stats(xa[:], xk)
                ln_normalize(xa[:], xk, xa[:], xk)
                mod_transpose(xa[:], xk, sc2, sh2, lambda k, tb=tb: u2T[:, k, tb * 128:(tb + 1) * 128], "u2T",
                              dst32=lambda k: u32[:, k, :], d32k="u32")
                psr, pkr = psn()
                for k in range(KC):
                    S.op("pe", lambda e, k=k: e.matmul(psr[:, 0:36], u32[:, k, :], wr_sb[:, k, :], start=(k == 0),
                                                       stop=(k == KC - 1)), r=["u32", "wr"], w=[pkr])
                lg = rt[:, 0:36]
                S.op("dve", lambda e: e.tensor_tensor(out=lg, in0=psr[:, 0:36], in1=brb, op=ALU.add),
                     r=[pkr, "small"], w=["rt"])
                gmax = rt[:, 36:37]
                S.op("dve", lambda e: e.reduce_max(out=gmax, in_=rt[:, 0:4], axis=AX.X), r=["rt"], w=["rt"])
                oh = rt[:, 40:44]
                S.op("dve", lambda e: e.tensor_scalar(out=oh, in0=rt[:, 0:4], scalar1=gmax, scalar2=None,
                                                      op0=ALU.is_ge), r=["rt"], w=["rt"])
                ge = rt[:, 44:48]
                S.op("dve", lambda e: e.tensor_scalar(out=ge, in0=rt[:, 0:4], scalar1=gmax, scalar2=None,
                                                      op0=ALU.subtract), r=["rt"], w=["rt"])
                gs = rt[:, 37:38]
                S.op("act", lambda e: e.activation(out=ge, in_=ge, func=AF.Exp, accum_out=gs), r=["rt"], w=["rt"])
                el = rt[:, 48:56]
                msk = rt[:, 56:88]
                S.op("dve", lambda e: e.tensor_tensor(out=msk.rearrange("p (g x) -> p g x", g=4),
                                                      in0=rt[:, 4:36].rearrange("p (g x) -> p g x", g=4),
                                                      in1=oh.unsqueeze(2).to_broadcast([128, 4, 8]), op=ALU.mult),
                     r=["rt"], w=["rt"])
                S.op("dve", lambda e: e.reduce_sum(out=el, in_=msk.rearrange("p (g x) -> p x g", g=4), axis=AX.X),
                     r=["rt"], w=["rt"])
                top8 = rt[:, 88:96]
                S.op("dve", lambda e: e.max(out=top8, in_=el), r=["rt"], w=["rt"])
                ee = rt[:, 96:104]
                S.op("dve", lambda e: e.tensor_scalar(out=ee, in0=el, scalar1=top8[:, 0:1], scalar2=None,
                                                      op0=ALU.subtract), r=["rt"], w=["rt"])
                S.op("act", lambda e: e.activation(out=ee, in_=ee, func=AF.Exp), r=["rt"], w=["rt"])
                m2 = rt[:, 104:112]
                S.op("dve", lambda e: e.tensor_scalar(out=m2, in0=el, scalar1=top8[:, 1:2], scalar2=None,
                                                      op0=ALU.is_ge), r=["rt"], w=["rt"])
                S.op("dve", lambda e: e.tensor_tensor(out=ee, in0=ee, in1=m2, op=ALU.mult), r=["rt"], w=["rt"])
                den = rt[:, 38:39]
                S.op("dve", lambda e: e.reduce_sum(out=den, in_=ee, axis=AX.X), r=["rt"], w=["rt"])
                S.op("dve", lambda e: e.tensor_tensor(out=den, in0=den, in1=gs, op=ALU.mult), r=["rt"], w=["rt"])
                S.op("dve", lambda e: e.reciprocal(out=den, in_=den), r=["rt"], w=["rt"])
                S.op("dve", lambda e: e.tensor_scalar(out=ee, in0=ee, scalar1=den, scalar2=None, op0=ALU.mult),
                     r=["rt"], w=["rt"])
                S.op("dve", lambda e, tb=tb: e.tensor_tensor(
                    out=comb[:, tb, :].rearrange("p (g x) -> p g x", g=4),
                    in0=oh.unsqueeze(2).to_broadcast([128, 4, 8]), in1=ee.unsqueeze(1).to_broadcast([128, 4, 8]),
                    op=ALU.mult), r=["rt"], w=["comb"])
            S.op("pool", lambda e: e.memset(yacc[:], 0.0), w=["yacc"])
            for ex in range(NEXP):
                for j in range(4):
                    wp, wk = load_panel(wexp[ex, j])
                    for sbk in range(GT // 512):
                        tk0 = sbk * 512
                        psg, pkg = psn()
                        for k in range(KC):
                            S.op("pe", lambda e, k=k: e.matmul(psg[:], wp[:, k, 0:128], u2T[:, k, tk0:tk0 + 512],
                                                               start=(k == 0), stop=(k == KC - 1)),
                                 r=[wk, "u2T"], w=[pkg])
                        psu_, pku_ = psn()
                        for k in range(KC):
                            S.op("pe", lambda e, k=k: e.matmul(psu_[:], wp[:, k, 128:256], u2T[:, k, tk0:tk0 + 512],
                                                               start=(k == 0), stop=(k == KC - 1)),
                                 r=[wk, "u2T"], w=[pku_])
                        sg, sgk = t32()
                        S.op("act", lambda e: e.activation(out=sg[:], in_=psg[:], func=AF.Silu), r=[pkg], w=[sgk])
                        S.op("dve", lambda e, j=j: e.tensor_tensor(out=actT[:, j, tk0:tk0 + 512], in0=sg[:],
                                                                   in1=psu_[:], op=ALU.mult),
                             r=[sgk, pku_], w=["actT"])
                for dh in range(2):
                    wd, wdk = load_panel(wexp[ex, 4 + dh])
                    for tb in range(GTB):
                        for dq in range(2):
                            psy, pky = psn()
                            for j in range(4):
                                S.op("pe", lambda e, j=j, dq=dq, tb=tb: e.matmul(
                                    psy[:].rearrange("p (a b) -> p a b", a=2),
                                    actT[:, j, tb * 128:(tb + 1) * 128],
                                    wd[:, j * 4 + dq * 2:j * 4 + dq * 2 + 2, :], start=(j == 0), stop=(j == 3)),
                                    r=[wdk, "actT"], w=[pky])
                            c0 = dh * 1024 + dq * 512
                            S.op("dve", lambda e, tb=tb, c0=c0: e.scalar_tensor_tensor(
                                out=yacc[:, tb, c0:c0 + 512], in0=psy[:], scalar=comb[:, tb, ex:ex + 1],
                                in1=yacc[:, tb, c0:c0 + 512], op0=ALU.mult, op1=ALU.add),
                                r=[pky, "comb", "yacc"], w=["yacc"])
            for tb in range(GTB):
                tok0 = gI * GT + tb * 128
                xa = XT[xti[0]]
                xk = f"xt{xti[0]}"
                xti[0] ^= 1
                load_x1(tok0, xa, xk)
                S.op("dve", lambda e, tb=tb: e.tensor_tensor(out=yacc[:, tb, :], in0=yacc[:, tb, :], in1=g2bc[:],
                                                             op=ALU.mult), r=["yacc", "g2bc"], w=["yacc"])
                S.op("dve", lambda e, tb=tb: e.scalar_tensor_tensor(out=xa[:], in0=xa[:], scalar=ALPHA,
                                                                    in1=yacc[:, tb, :], op0=ALU.mult, op1=ALU.add),
                     r=[xk, "yacc"], w=[xk])
                ln_stats(xa[:], xk)
                ln_normalize(xa[:], xk, xa[:], xk)
                S.op("dve", lambda e: e.tensor_tensor(out=xa[:], in0=xa[:], in1=lnb[:, 0, :], op=ALU.mult),
                     r=[xk, "lnb"], w=[xk])
                S.op("pool", lambda e: e.tensor_tensor(out=xa[:], in0=xa[:], in1=lnb[:, 1, :], op=ALU.add),
                     r=[xk, "lnb"], w=[xk])
                S.dma("sp", out[tok0:tok0 + 128, :], xa[:], f"ost{xk}", r=[xk], w=["out"])
        for slot, (s, sid, val) in S.slots.items():
            nc.sync.wait_ge(s, val)
        DEBUG["ninst"] = S.ninst
        es2.close()
    return nc, list(dbg_out)


def _panelize(w, ncols_total=None):
    K, C = w.shape
    assert K == D and C % PW == 0
    return np.ascontiguousarray(w.reshape(KC, 128, C // PW, PW).transpose(2, 1, 0, 3))


def _consts(flag):
    c = np.zeros((128, 5 * 128 + 4), np.float32)
    t = np.arange(128)
    same = (t[:, None] // 64) == (t[None, :] // 64)
    c[:, 0:128] = np.eye(128)
    c[:, 128:256] = (same & (t[:, None] <= t[None, :]))
    c[:, 256:384] = same
    c[:, 384:512] = np.where(same & (t[None, :] >= t[:, None]), 0.0, NEG)
    c[:, 512:640] = (same & (t[None, :] > t[:, None]))
    c[:, 640] = flag
    c[:, 641] = (flag - 1.0) * 30000.0
    c[:64, 642] = 1.0
    c[64:, 643] = 1.0
    return c


def prepare(inputs, n_cores=8):
    f = lambda a: np.asarray(a, dtype=np.float32)
    x = f(inputs["x"])
    B, SEQ, _ = x.shape
    HALF = SEQ // 2
    w_in = f(inputs["w_in"])[0]
    qperm = np.zeros(1024, np.int64)
    for kvh in range(4):
        for cc in range(2):
            for half in range(2):
                dst = (2 * kvh + cc) * 128 + half * 64
                src = (4 * kvh + 2 * half + cc) * 64
                qperm[dst:dst + 64] = np.arange(src, src + 64)
    kdup = np.concatenate([np.concatenate([np.arange(1024 + h * 64, 1024 + h * 64 + 64)] * 2) for h in range(4)])
    gcols = np.concatenate([np.concatenate([np.arange(5648 + i * 128, 5648 + (i + 1) * 128),
                                            np.arange(7696 + i * 128, 7696 + (i + 1) * 128)]) for i in range(16)])
    cols = np.concatenate([qperm, kdup, np.arange(1280, 1536), np.arange(1536, 5632), gcols])
    win = _panelize(w_in[:, cols])
    wbd = np.ascontiguousarray(w_in[:, 5632:5648].reshape(KC, 128, 16).transpose(1, 0, 2))
    wada = _panelize(f(inputs["w_ada"])[0])
    b_ada = f(inputs["b_ada"])[0]
    badaT = np.ascontiguousarray(b_ada.reshape(96, 128).T)
    badag = np.ascontiguousarray(np.stack([b_ada[4096:6144], b_ada[10240:12288]]))
    convw = np.ascontiguousarray(f(inputs["conv_w"])[0].reshape(4, 24, 128).transpose(2, 1, 0))
    wpa = f(inputs["w_proj_a"])[0]
    wpb = f(inputs["w_proj_b"])[0]
    wproj = np.ascontiguousarray(np.concatenate([wpa, wpb], 0).reshape(KC, 128, 8, PW).transpose(2, 1, 0, 3))
    wout = _panelize(f(inputs["w_out"])[0])
    lnv = np.ascontiguousarray(np.stack([f(inputs["ln1_g"])[0], f(inputs["ln1_b"])[0], f(inputs["ln2_g"])[0],
                                         f(inputs["ln2_b"])[0]]))
    wrt = np.concatenate([f(inputs["w_router_group"])[0], f(inputs["w_router_expert"])[0]], 1)
    wr = np.ascontiguousarray(wrt.reshape(KC, 128, 36).transpose(1, 0, 2))
    br = np.concatenate([f(inputs["b_router_group"])[0], f(inputs["b_router_expert"])[0]])
    wgu = f(inputs["w_gate_up"])[0]
    wdn = f(inputs["w_down"])[0]
    wexp = np.empty((NEXP, 6, 128, KC, PW), np.float32)
    gu = wgu.reshape(NEXP, KC, 128, 2, 4, 128)
    wexp[:, 0:4] = gu.transpose(0, 4, 2, 1, 3, 5).reshape(NEXP, 4, 128, KC, PW)
    dn = wdn.reshape(NEXP, 4, 128, 2, 4, PW)
    wexp[:, 4:6] = dn.transpose(0, 3, 2, 1, 4, 5).reshape(NEXP, 2, 128, KC, PW)
    shared = dict(wada=wada, badaT=badaT, badag=badag, win=win, wbd=wbd, convw=convw,
                  sinks=f(inputs["swa_sinks"])[0], alog=f(inputs["gdn_a_log"])[0], dtb=f(inputs["gdn_dt_bias"])[0],
                  normw=f(inputs["gdn_norm_w"])[0], wproj=wproj, wout=wout, lnv=lnv, wr=wr, br=br, wexp=wexp)
    c = f(inputs["c"])
    in_maps = []
    for core in range(n_cores):
        b, h = core // 2, core % 2
        m = dict(shared)
        m["xm"] = np.ascontiguousarray(x[b, h * HALF:(h + 1) * HALF])
        m["xp"] = np.ascontiguousarray(x[b, 0:HALF]) if h == 1 else np.zeros((HALF, D), np.float32)
        m["cT"] = np.ascontiguousarray(c[b].reshape(KC, 128).T)
        m["consts"] = _consts(float(h))
        in_maps.append(m)
    return in_maps, (B, SEQ, HALF)


_CACHE = {}


def kernel(**inputs):
    in_maps, (B, SEQ, HALF) = prepare(inputs)
    nsb = HALF // 512
    key = (nsb,)
    if key not in _CACHE:
        _CACHE[key] = build_nc(nsb, nsb, 2 if nsb >= 4 else 1)[0]
    nc = _CACHE[key]
    res = run_bass_kernel_spmd(nc, in_maps, core_ids=list(range(8)))
    outp = np.empty((B, SEQ, D), np.float32)
    for core in range(8):
        b, h = core // 2, core % 2
        outp[b, h * HALF:(h + 1) * HALF] = res.results[core]["out"]
    return outp
```
